# Optimizing a Trainium2 kernel written in Bass

```python
import math
import jax, jax.numpy as jnp
from jax import lax
import numpy as np

D_MODEL = 2048
BATCH = 8
SEQ = 2048
DEPTH = 1

ATTN_WIDTH = D_MODEL // 2
SSM_WIDTH = D_MODEL // 2
HEAD_DIM = 64
N_Q_HEADS = ATTN_WIDTH // HEAD_DIM
N_KV_HEADS = N_Q_HEADS // 4
KV_WIDTH = N_KV_HEADS * HEAD_DIM
WINDOW = 128
BLOCK = WINDOW
ROPE_THETA = 500000.0
ROPE_DIMS = HEAD_DIM // 4
SSM_GROUP = 16
N_SSM_GROUPS = SSM_WIDTH // SSM_GROUP
SSM_STATE = 64
DT_MIN = 1e-3
DT_MAX = 1e-1
N_BRANCHES = 2
IN_COLS = ATTN_WIDTH + 2 * KV_WIDTH + SSM_WIDTH + N_BRANCHES * D_MODEL
PEER_HEADS = 8
PEER_NKEYS = 128
PEER_N = PEER_NKEYS * PEER_NKEYS
PEER_QDIM = 256
PEER_HALF = PEER_QDIM // 2
PEER_TOPK = 16
PEER_CHUNK = 128
RMS_EPS = 1e-6
MASK_VALUE = -1e30

kernel_name = 'hybrid_swa_s5_peer_adaln_block'


def rmsnorm(x, g):
    xf = x.astype(jnp.float32)
    y = xf * lax.rsqrt(jnp.mean(xf * xf, axis=-1, keepdims=True) + RMS_EPS)
    return (y * g.astype(jnp.float32)).astype(x.dtype)


def modulate(h, shift, scale):
    return h * (1.0 + scale[:, None, :]) + shift[:, None, :]


def rope_partial(t, positions):
    inv_freq = ROPE_THETA ** (-(jnp.arange(0, ROPE_DIMS, 2, dtype=jnp.float32) / ROPE_DIMS))
    ang = positions.astype(jnp.float32)[..., None] * inv_freq
    cos = jnp.cos(ang)[:, :, None, :]
    sin = jnp.sin(ang)[:, :, None, :]
    tr = t[..., :ROPE_DIMS].astype(jnp.float32)
    t1, t2 = tr[..., :ROPE_DIMS // 2], tr[..., ROPE_DIMS // 2:]
    rot = jnp.concatenate([t1 * cos - t2 * sin, t2 * cos + t1 * sin], axis=-1)
    return jnp.concatenate([rot.astype(t.dtype), t[..., ROPE_DIMS:]], axis=-1)


def sliding_window_attention(q, k, v, sinks):
    b, s = q.shape[0], q.shape[1]
    nb = s // BLOCK
    rep = N_Q_HEADS // N_KV_HEADS
    qb = q.reshape(b, nb, BLOCK, N_KV_HEADS, rep, HEAD_DIM)

    def band(t):
        tp = jnp.pad(t, ((0, 0), (BLOCK, 0), (0, 0), (0, 0)))
        cur = tp[:, BLOCK:].reshape(b, nb, BLOCK, N_KV_HEADS, HEAD_DIM)
        prev = tp[:, :-BLOCK].reshape(b, nb, BLOCK, N_KV_HEADS, HEAD_DIM)
        return jnp.concatenate([prev, cur], axis=2)

    kb, vb = band(k), band(v)
    scores = jnp.einsum('bnqgrd,bnkgd->bngrqk', qb, kb,
                        preferred_element_type=jnp.float32) * (HEAD_DIM ** -0.5)
    qi = jnp.arange(BLOCK)[:, None] + BLOCK
    kj = jnp.arange(2 * BLOCK)[None, :]
    diff = qi - kj
    valid = (diff >= 0) & (diff < WINDOW)
    blk = jnp.arange(nb)[:, None, None]
    valid = valid[None] & ~((blk == 0) & (kj[None] < BLOCK))
    scores = jnp.where(valid[None, :, None, None], scores, MASK_VALUE)
    sink = sinks.astype(jnp.float32).reshape(N_KV_HEADS, rep)[None, None, :, :, None, None]
    sink = jnp.broadcast_to(sink, scores.shape[:-1] + (1,))
    probs = jax.nn.softmax(jnp.concatenate([scores, sink], axis=-1), axis=-1)[..., :-1]
    out = jnp.einsum('bngrqk,bnkgd->bnqgrd', probs.astype(v.dtype), vb)
    return out.reshape(b, s, N_Q_HEADS * HEAD_DIM)


def s5_ssm(u, A_re, A_im, log_dt, B_re, B_im, C_re, C_im, D_skip, w_glu, b_glu):
    b, s, _ = u.shape
    uf = u.astype(jnp.float32).reshape(b, s, N_SSM_GROUPS, SSM_GROUP)
    dt = jnp.exp(log_dt.astype(jnp.float32))[:, None]
    lam_re = jnp.minimum(A_re.astype(jnp.float32), -1e-4)
    lam_im = A_im.astype(jnp.float32)
    mag = jnp.exp(lam_re * dt)
    ab_re = mag * jnp.cos(lam_im * dt)
    ab_im = mag * jnp.sin(lam_im * dt)
    den = lam_re * lam_re + lam_im * lam_im
    num_re = ab_re - 1.0
    z_re = (num_re * lam_re + ab_im * lam_im) / den
    z_im = (ab_im * lam_re - num_re * lam_im) / den
    bre = B_re.astype(jnp.float32)
    bim = B_im.astype(jnp.float32)
    bb_re = z_re[..., None] * bre - z_im[..., None] * bim
    bb_im = z_re[..., None] * bim + z_im[..., None] * bre
    bu_re = jnp.einsum('bsgh,gph->bsgp', uf, bb_re)
    bu_im = jnp.einsum('bsgh,gph->bsgp', uf, bb_im)
    a_re = jnp.broadcast_to(ab_re[None, None], (1, s, N_SSM_GROUPS, SSM_STATE))
    a_im = jnp.broadcast_to(ab_im[None, None], (1, s, N_SSM_GROUPS, SSM_STATE))

    def combine(left, right):
        a1r, a1i, b1r, b1i = left
        a2r, a2i, b2r, b2i = right
        return (a2r * a1r - a2i * a1i,
                a2r * a1i + a2i * a1r,
                a2r * b1r - a2i * b1i + b2r,
                a2r * b1i + a2i * b1r + b2i)

    _, _, x_re, x_im = lax.associative_scan(combine, (a_re, a_im, bu_re, bu_im), axis=1)
    y = (jnp.einsum('bsgp,ghp->bsgh', x_re, C_re.astype(jnp.float32))
         - jnp.einsum('bsgp,ghp->bsgh', x_im, C_im.astype(jnp.float32))
         + D_skip.astype(jnp.float32).reshape(N_SSM_GROUPS, SSM_GROUP) * uf)
    y = y.reshape(b, s, SSM_WIDTH)
    z = jax.nn.gelu(y)
    out = z * jax.nn.sigmoid(z @ w_glu.astype(jnp.float32) + b_glu.astype(jnp.float32))
    return out.astype(u.dtype)


def peer(h, w_query, sub_keys, expert_down, expert_up):
    b, s, d = h.shape
    t = b * s
    hf = h.reshape(t, d)
    q = (hf @ w_query).reshape(t, PEER_HEADS, 2, PEER_HALF)
    sc = jnp.einsum('thcd,hckd->thck', q, sub_keys, preferred_element_type=jnp.float32)
    s_top, i_top = lax.top_k(sc, PEER_TOPK)
    cand = s_top[:, :, 0, :, None] + s_top[:, :, 1, None, :]
    cand_idx = i_top[:, :, 0, :, None] * PEER_NKEYS + i_top[:, :, 1, None, :]
    best, sel = lax.top_k(cand.reshape(t, PEER_HEADS, PEER_TOPK * PEER_TOPK), PEER_TOPK)
    idx = jnp.take_along_axis(cand_idx.reshape(t, PEER_HEADS, PEER_TOPK * PEER_TOPK), sel, axis=-1)
    gates = jax.nn.softmax(best, axis=-1)

    def chunk_fn(args):
        xc, ic, gc = args
        u = jnp.take(expert_down, ic, axis=0)
        a = jnp.einsum('td,thkd->thk', xc, u)
        w = jax.nn.gelu(a.astype(jnp.float32)) * gc
        vv = jnp.take(expert_up, ic, axis=0)
        return jnp.einsum('thk,thkd->td', w.astype(xc.dtype), vv)

    nc = t // PEER_CHUNK
    out = lax.map(chunk_fn, (hf.reshape(nc, PEER_CHUNK, d),
                             idx.reshape(nc, PEER_CHUNK, PEER_HEADS, PEER_TOPK),
                             gates.reshape(nc, PEER_CHUNK, PEER_HEADS, PEER_TOPK)))
    return out.reshape(b, s, d)


def hybrid_layer(x, c, positions, w_ada, b_ada, g_mix, w_in, b_in, attn_sinks, w_attn_branch,
                 ssm_A_re, ssm_A_im, ssm_log_dt, ssm_B_re, ssm_B_im, ssm_C_re, ssm_C_im, ssm_D,
                 w_glu, b_glu, w_ssm_branch, w_out, g_ffn, w_query, sub_keys, expert_down, expert_up):
    b, s, _ = x.shape
    mod = jax.nn.silu(c) @ w_ada + b_ada
    shift1, scale1, gate1, shift2, scale2, gate2 = jnp.split(mod, 6, axis=-1)

    h = modulate(rmsnorm(x, g_mix), shift1, scale1)
    proj = h @ w_in + b_in
    o1 = ATTN_WIDTH
    o2 = o1 + KV_WIDTH
    o3 = o2 + KV_WIDTH
    o4 = o3 + SSM_WIDTH
    q, k, v, u, gate_logits = jnp.split(proj, [o1, o2, o3, o4], axis=-1)
    q = rope_partial(q.reshape(b, s, N_Q_HEADS, HEAD_DIM), positions)
    k = rope_partial(k.reshape(b, s, N_KV_HEADS, HEAD_DIM), positions)
    v = v.reshape(b, s, N_KV_HEADS, HEAD_DIM)
    y_attn = sliding_window_attention(q, k, v, attn_sinks) @ w_attn_branch
    y_ssm = s5_ssm(u, ssm_A_re, ssm_A_im, ssm_log_dt, ssm_B_re, ssm_B_im, ssm_C_re, ssm_C_im,
                   ssm_D, w_glu, b_glu) @ w_ssm_branch
    g = jax.nn.sigmoid(gate_logits.reshape(b, s, N_BRANCHES, D_MODEL))
    merged = g[:, :, 0] * y_attn + g[:, :, 1] * y_ssm
    x = x + gate1[:, None, :] * (merged @ w_out)

    h2 = modulate(rmsnorm(x, g_ffn), shift2, scale2)
    x = x + gate2[:, None, :] * peer(h2, w_query, sub_keys, expert_down, expert_up)
    return x


def setup_inputs(seed: int = 0) -> dict:
    key = jax.random.key(seed)
    ks = jax.random.split(key, 32)
    f32 = jnp.float32

    def nrm(k, shape, scale):
        return jax.random.normal(k, shape, f32) * scale

    L = DEPTH
    x = nrm(ks[0], (BATCH, SEQ, D_MODEL), 1.0)
    c = nrm(ks[1], (BATCH, D_MODEL), 1.0)
    offset = jax.random.randint(ks[2], (BATCH,), 0, 4096, dtype=jnp.int32)
    positions = offset[:, None] + jnp.arange(SEQ, dtype=jnp.int32)[None, :]
    n_idx = jnp.arange(SSM_STATE, dtype=f32)
    return {
        'x': x,
        'c': c,
        'positions': positions,
        'w_ada': nrm(ks[3], (L, D_MODEL, 6 * D_MODEL), 0.5 * D_MODEL ** -0.5),
        'b_ada': nrm(ks[4], (L, 6 * D_MODEL), 0.02),
        'g_mix': 1.0 + nrm(ks[5], (L, D_MODEL), 0.02),
        'w_in': nrm(ks[6], (L, D_MODEL, IN_COLS), D_MODEL ** -0.5),
        'b_in': nrm(ks[7], (L, IN_COLS), 0.02),
        'attn_sinks': nrm(ks[8], (L, N_Q_HEADS), 0.5),
        'w_attn_branch': nrm(ks[9], (L, ATTN_WIDTH, D_MODEL), ATTN_WIDTH ** -0.5),
        'ssm_A_re': -0.5 + nrm(ks[10], (L, N_SSM_GROUPS, SSM_STATE), 0.01),
        'ssm_A_im': math.pi * n_idx + nrm(ks[11], (L, N_SSM_GROUPS, SSM_STATE), 0.01),
        'ssm_log_dt': jax.random.uniform(ks[12], (L, N_SSM_GROUPS), f32, math.log(DT_MIN), math.log(DT_MAX)),
        'ssm_B_re': nrm(ks[13], (L, N_SSM_GROUPS, SSM_STATE, SSM_GROUP), (2.0 * SSM_GROUP) ** -0.5),
        'ssm_B_im': nrm(ks[14], (L, N_SSM_GROUPS, SSM_STATE, SSM_GROUP), (2.0 * SSM_GROUP) ** -0.5),
        'ssm_C_re': nrm(ks[15], (L, N_SSM_GROUPS, SSM_GROUP, SSM_STATE), (2.0 * SSM_STATE) ** -0.5),
        'ssm_C_im': nrm(ks[16], (L, N_SSM_GROUPS, SSM_GROUP, SSM_STATE), (2.0 * SSM_STATE) ** -0.5),
        'ssm_D': nrm(ks[17], (L, SSM_WIDTH), 1.0),
        'w_glu': nrm(ks[18], (L, SSM_WIDTH, SSM_WIDTH), SSM_WIDTH ** -0.5),
        'b_glu': nrm(ks[19], (L, SSM_WIDTH), 0.02),
        'w_ssm_branch': nrm(ks[20], (L, SSM_WIDTH, D_MODEL), SSM_WIDTH ** -0.5),
        'w_out': nrm(ks[21], (L, D_MODEL, D_MODEL), D_MODEL ** -0.5),
        'g_ffn': 1.0 + nrm(ks[22], (L, D_MODEL), 0.02),
        'w_query': nrm(ks[23], (L, D_MODEL, PEER_HEADS * PEER_QDIM), D_MODEL ** -0.5),
        'sub_keys': nrm(ks[24], (L, PEER_HEADS, 2, PEER_NKEYS, PEER_HALF), PEER_HALF ** -0.5),
        'expert_down': nrm(ks[25], (L, PEER_N, D_MODEL), D_MODEL ** -0.5),
        'expert_up': nrm(ks[26], (L, PEER_N, D_MODEL), 0.5),
        'g_final': 1.0 + nrm(ks[27], (D_MODEL,), 0.02),
    }


def reference(x, c, positions, w_ada, b_ada, g_mix, w_in, b_in, attn_sinks, w_attn_branch,
              ssm_A_re, ssm_A_im, ssm_log_dt, ssm_B_re, ssm_B_im, ssm_C_re, ssm_C_im, ssm_D,
              w_glu, b_glu, w_ssm_branch, w_out, g_ffn, w_query, sub_keys, expert_down, expert_up,
              g_final):
    for layer in range(DEPTH):
        x = hybrid_layer(x, c, positions, w_ada[layer], b_ada[layer], g_mix[layer], w_in[layer],
                         b_in[layer], attn_sinks[layer], w_attn_branch[layer], ssm_A_re[layer],
                         ssm_A_im[layer], ssm_log_dt[layer], ssm_B_re[layer], ssm_B_im[layer],
                         ssm_C_re[layer], ssm_C_im[layer], ssm_D[layer], w_glu[layer], b_glu[layer],
                         w_ssm_branch[layer], w_out[layer], g_ffn[layer], w_query[layer],
                         sub_keys[layer], expert_down[layer], expert_up[layer])
    return rmsnorm(x, g_final)
```

```python
import math
import os
from contextlib import ExitStack

import numpy as np
import concourse.bass as bass
import concourse.mybir as mybir
from concourse.bass_utils import run_bass_kernel_spmd

F32 = mybir.dt.float32
BF16 = mybir.dt.bfloat16
I32 = mybir.dt.int32
U32 = mybir.dt.uint32
AF = mybir.ActivationFunctionType
ALU = mybir.AluOpType

STAGES_DONE = ["adaLN-mod", "norm1+modulate", "in-proj(q,k,v,u,gates)", "rope", "swa-attention+sinks", "s5-ssm+glu",
               "gated-merge", "w_out+gate1+residual", "final-rmsnorm"]
STAGES_MISSING = []

D_MODEL = 2048
SEQ = 2048
N_CORES = 8
RMS_EPS = 1e-6
P = 128
NT = SEQ // P
KC = D_MODEL // P
MT = 512
NM = SEQ // MT
IN_COLS = 6656
ROPE_THETA = 500000.0
TWO_PI = 2.0 * math.pi

ENGS = ("sync", "tensor", "vector", "scalar", "gpsimd")
N_DMA_SEMS = 12


class Prog:
    def __init__(self, nc, stack):
        self.nc = nc
        self.stack = stack
        self.ins = []
        self.per_eng = {e: [] for e in ENGS}
        self.res = {}
        self.out_dmas = []

    def _deps(self, reads, writes):
        deps = set()
        for k in reads:
            r = self.res.get(k)
            if r and r[0] is not None:
                deps.add(r[0])
        for k in writes:
            r = self.res.get(k)
            if r:
                if r[0] is not None:
                    deps.add(r[0])
                deps.update(r[1])
        return deps

    def _commit(self, iid, reads, writes):
        for k in reads:
            self.res.setdefault(k, [None, set()])[1].add(iid)
        for k in writes:
            self.res[k] = [iid, set()]

    def emit(self, eng, fn, reads=(), writes=()):
        deps = self._deps(reads, writes)
        iid = len(self.ins)
        if eng == "tensor":
            deps = {d for d in deps if self.ins[d]["eng"] != "tensor" or self.ins[d]["dma"]}
        self.ins.append(dict(eng=eng, fn=fn, deps=deps, dma=False, waited=False))
        self.per_eng[eng].append(iid)
        self._commit(iid, reads, writes)
        return iid

    def dma(self, eng, out, in_, reads=(), writes=(), is_output=False, **kw):
        deps = self._deps(reads, writes)
        iid = len(self.ins)
        fn = lambda e, out=out, in_=in_, kw=kw: e.dma_start(out=out, in_=in_, **kw)
        self.ins.append(dict(eng=eng, fn=fn, deps=deps, dma=True, waited=False))
        self.per_eng[eng].append(iid)
        self._commit(iid, reads, writes)
        if is_output:
            self.out_dmas.append(iid)
        return iid

    def dmaf(self, eng, fn, reads=(), writes=(), is_output=False):
        deps = self._deps(reads, writes)
        iid = len(self.ins)
        self.ins.append(dict(eng=eng, fn=fn, deps=deps, dma=True, waited=False, sw=True))
        self.per_eng[eng].append(iid)
        self._commit(iid, reads, writes)
        if is_output:
            self.out_dmas.append(iid)
        return iid

    def finish(self):
        nc = self.nc
        ins = self.ins
        fin = len(ins)
        ins.append(dict(eng="sync", fn=None, deps=set(self.out_dmas), dma=False, waited=False))
        self.per_eng["sync"].append(fin)
        for r in ins:
            for d in r["deps"]:
                ins[d]["waited"] = True
        EPOCH = 2000
        DEPOCH = 120
        sems = {}

        def sem_of(key):
            if key not in sems:
                sems[key] = self.stack.enter_context(nc.semaphore("m_" + "_".join(str(k) for k in key)))
            return sems[key]

        ecnt = {e: 0 for e in ENGS}
        ngather = [0]
        duse = [0] * N_DMA_SEMS
        dlast = [None] * N_DMA_SEMS
        nd = 0
        for iid, r in enumerate(ins):
            if r["dma"] and r.get("sw"):
                ep, u = divmod(ngather[0], 200)
                ngather[0] += 1
                r["ev"] = (("g", ep), 16 * (u + 1))
            elif r["dma"]:
                slot = nd % N_DMA_SEMS
                nd += 1
                if dlast[slot] is not None:
                    r["deps"].add(dlast[slot])
                    ins[dlast[slot]]["waited"] = True
                dlast[slot] = iid
                ep, u = divmod(duse[slot], DEPOCH)
                duse[slot] += 1
                r["ev"] = (("d", slot, ep), 16 * (u + 1))
            elif r["waited"]:
                ep, u = divmod(ecnt[r["eng"]], EPOCH)
                ecnt[r["eng"]] += 1
                r["ev"] = (("e", r["eng"], ep), u + 1)
            else:
                r["ev"] = None
        self.stats = dict(n_ins=len(ins), ecnt=dict(ecnt), duse=list(duse))

        with nc.Block() as block:
            def make(engname):
                def body(eng):
                    seen = {}
                    for iid in self.per_eng[engname]:
                        r = ins[iid]
                        need = {}
                        for d in r["deps"]:
                            key, val = ins[d]["ev"]
                            if seen.get(key, 0) >= val:
                                continue
                            need[key] = max(need.get(key, 0), val)
                        for key, val in need.items():
                            eng.wait_ge(sem_of(key), val)
                            seen[key] = val
                        if r["fn"] is None:
                            continue
                        bi = r["fn"](eng)
                        if r["ev"] is not None:
                            key, val = r["ev"]
                            bi.then_inc(sem_of(key), 16 if key[0] in ("d", "g") else 1)
                return body
            block.sync(make("sync"))
            block.tensor(make("tensor"))
            block.vector(make("vector"))
            block.scalar(make("scalar"))
            block.gpsimd(make("gpsimd"))


def host_consts():
    c = {}
    c["ident_f"] = np.eye(P, dtype=np.float32)
    pm = np.zeros((P, P), np.float32)
    for h0 in (0, 64):
        for d in range(8):
            pm[h0 + d + 8, h0 + d] = -1.0
            pm[h0 + d, h0 + d + 8] = 1.0
    c["rope_pm"] = pm
    invf = np.zeros((P, 1), np.float32)
    f = ROPE_THETA ** (-(np.arange(0, 16, 2, dtype=np.float32) / 16.0))
    for h0 in (0, 64):
        for d in range(16):
            invf[h0 + d, 0] = f[d % 8]
    c["rope_invf"] = invf.astype(np.float32)
    tk = np.arange(P)[:, None]; tq = np.arange(P)[None, :]
    rm = np.zeros((P, 2), np.float32); rm[:64, 0] = 1.0; rm[64:, 1] = 1.0
    c["rowmask"] = rm
    c["ones_f"] = np.ones((P, P), np.float32)
    c["iota16"] = np.tile(np.arange(16, dtype=np.float32)[None, :], (P, 1))
    c["mask_cur"] = (tk <= tq).astype(np.float32)
    c["mask_prev"] = (tk > tq).astype(np.float32)
    return c


def build_nc(debug=False, upto=9):
    nc = bass.Bass("TRN2", target_bir_lowering=False)

    def din(name, shape, dt=F32):
        return nc.dram_tensor(name, list(shape), dt, kind="ExternalInput").ap()

    x = din("x", [SEQ, D_MODEL])
    pos = din("pos", [1, SEQ], I32)
    c_col = din("c_col", [P, KC])
    w_ada = din("w_ada", [D_MODEL, 6 * D_MODEL])
    b_ada_col = din("b_ada_col", [P, 96])
    g_mix_col = din("g_mix_col", [P, KC])
    w_in = din("w_in", [D_MODEL, IN_COLS])
    b_in_col = din("b_in_col", [P, 56])
    g_final = din("g_final", [1, D_MODEL])
    ident_f_d = din("ident_f", [P, P])
    rope_pm_d = din("rope_pm", [P, P])
    rope_invf_d = din("rope_invf", [P, 1])
    mask_cur_d = din("mask_cur", [P, P])
    mask_prev_d = din("mask_prev", [P, P])
    sinks_d = din("attn_sinks", [1, 16])
    rowmask_d = din("rowmask", [P, 2])
    ssm_are_d = din("ssm_are_l", [P, 32]); ssm_aim_d = din("ssm_aim_l", [P, 32]); ssm_ldt_d = din("ssm_ldt_l", [P, 32])
    ssm_bre_d = din("ssm_bre_l", [P, 32, 16]); ssm_bim_d = din("ssm_bim_l", [P, 32, 16])
    ssm_cre_d = din("ssm_cre_l", [P, 32, 16]); ssm_cim_d = din("ssm_cim_l", [P, 32, 16])
    ssm_d_col_d = din("ssm_d_col", [P, 8]); b_glu_col_d = din("b_glu_col", [P, 8])
    w_glu = din("w_glu", [1024, 1024])
    w_ab = din("w_attn_branch", [1024, D_MODEL]); w_sb = din("w_ssm_branch", [1024, D_MODEL])
    w_out = din("w_out", [D_MODEL, D_MODEL])
    ones_d = din("ones_f", [P, P])
    g_ffn_col_d = din("g_ffn_col", [P, KC])
    w_query = din("w_query", [D_MODEL, D_MODEL])
    sk_l_d = din("sub_keys_l", [P, 16, P])
    iota16_d = din("iota16", [P, 16])
    expert_down = din("expert_down", [16384, D_MODEL])
    expert_up = din("expert_up", [16384, D_MODEL])
    y = nc.dram_tensor("y", [SEQ, D_MODEL], F32, kind="ExternalOutput").ap()
    dbg = {}
    if debug:
        for nm, shp in (("d_mod", [P, 96]), ("d_hT", [P, 8, MT]), ("d_q", [P, 8, MT]),
                        ("d_k", [P, 4, MT]), ("d_v", [P, 2, MT]), ("d_u", [P, 8, MT]), ("d_att", [P, 8, MT]), ("d_abar", [P, 64]), ("d_y", [P, 8, MT]), ("d_ssm", [P, 8, MT]), ("d_mrg", [P, 8, MT]), ("d_x1", [P, D_MODEL]), ("d_eidx", [P, P]), ("d_gate", [P, P]), ("d_x2", [P, D_MODEL])):
            dbg[nm] = nc.dram_tensor(nm, shp, (I32 if nm == "d_eidx" else F32) if nm in ("d_mod", "d_abar", "d_x1", "d_eidx", "d_gate", "d_x2") else BF16, kind="ExternalOutput").ap()

    w_ada_v = w_ada.rearrange("(kc p) n -> p kc n", p=P)
    w_in_v = w_in.rearrange("(kc p) n -> p kc n", p=P)

    with ExitStack() as st:
        def sb(name, shape, dt=F32):
            return st.enter_context(nc.sbuf_tensor("sb_" + name, list(shape), dt))

        def ps(name, shape, dt=F32):
            return st.enter_context(nc.psum_tensor(name, list(shape), dt))

        pg = Prog(nc, st)
        E, D = pg.emit, pg.dma

        ident_f = sb("ident_f", [P, P]); D("sync", ident_f[:], ident_f_d, writes=["ident_f"])
        ident_b = sb("ident_b", [P, P], BF16)
        E("vector", lambda e: e.tensor_copy(out=ident_b[:], in_=ident_f[:]), ["ident_f"], ["ident_b"])
        pm_f = sb("pm_f", [P, P]); D("sync", pm_f[:], rope_pm_d, writes=["pm_f"])
        pm_b = sb("pm_b", [P, P], BF16)
        E("vector", lambda e: e.tensor_copy(out=pm_b[:], in_=pm_f[:]), ["pm_f"], ["pm_b"])
        invf = sb("invf", [P, 1]); D("sync", invf[:], rope_invf_d, writes=["invf"])
        ccol = sb("ccol", [P, KC]); D("sync", ccol[:], c_col, writes=["ccol"])
        bada = sb("bada", [P, 96]); D("sync", bada[:], b_ada_col, writes=["bada"])
        gmix = sb("gmix", [P, KC]); D("sync", gmix[:], g_mix_col, writes=["gmix"])
        binc = sb("binc", [P, 56]); D("sync", binc[:], b_in_col, writes=["binc"])

        mtmp = sb("mtmp", [P, P])
        mcur = sb("mcur", [P, P], BF16); mprev = sb("mprev", [P, P], BF16)
        D("sync", mtmp[:], mask_cur_d, writes=["mtmp"])
        E("vector", lambda e: e.tensor_copy(out=mcur[:], in_=mtmp[:]), ["mtmp"], ["mcur"])
        D("sync", mtmp[:], mask_prev_d, writes=["mtmp"])
        E("vector", lambda e: e.tensor_copy(out=mprev[:], in_=mtmp[:]), ["mtmp"], ["mprev"])
        rowmask = sb("rowmask", [P, 2]); D("sync", rowmask[:], rowmask_d, writes=["rowmask"])
        ones_f = sb("ones_f", [P, P]); D("sync", ones_f[:], ones_d, writes=["ones_f"])
        esink = sb("esink", [P, 16])
        D("sync", esink[:], sinks_d.partition_broadcast(P), writes=["esink"])
        E("scalar", lambda e: e.activation(out=esink[:], in_=esink[:], func=AF.Exp), ["esink"], ["esink"])
        psb = [ps("psb%d" % i, [P, 512]) for i in range(6)]
        psT = ps("psT", [P, 2048], BF16)
        psk = ["psb%d" % i for i in range(6)]

        silc = sb("silc", [P, KC])
        E("scalar", lambda e: e.activation(out=silc[:], in_=ccol[:], func=AF.Silu), ["ccol"], ["silc"])
        wa = [sb("wa%d" % i, [P, KC, 256]) for i in range(2)]
        modT = sb("modT", [P, 96])
        for blk in range(48):
            b = blk % 2
            D("sync", wa[b][:], w_ada_v[:, :, blk * 256:(blk + 1) * 256], writes=["wa%d" % b])
            for sub in range(2):
                j = blk * 2 + sub
                for kc in range(KC):
                    E("tensor", lambda e, b=b, sub=sub, j=j, kc=kc: e.matmul(
                        psb[0][:, j:j + 1], lhsT=wa[b][:, kc, sub * 128:(sub + 1) * 128],
                        rhs=silc[:, kc:kc + 1], start=(kc == 0), stop=(kc == KC - 1)),
                        ["wa%d" % b, "silc"], ["psb0"])
        E("vector", lambda e: e.tensor_tensor(out=modT[:], in0=psb[0][:, 0:96], in1=bada[:], op=ALU.add),
          ["psb0", "bada"], ["modT"])
        A1 = sb("A1", [P, KC])
        E("vector", lambda e: e.scalar_tensor_tensor(out=A1[:], in0=modT[:, 16:32], scalar=1.0, in1=gmix[:],
                                                     op0=ALU.add, op1=ALU.mult), ["modT", "gmix"], ["A1"])
        if debug:
            D("sync", dbg["d_mod"], modT[:], reads=["modT"], is_output=True)

        xb = [sb("xb%d" % i, [P, D_MODEL]) for i in range(2)]
        ss = [sb("ss%d" % i, [P, 1]) for i in range(2)]
        rstd = [sb("rstd%d" % i, [P, 1]) for i in range(2)]
        xnb = [sb("xnb%d" % i, [P, D_MODEL], BF16) for i in range(2)]
        hT = sb("hT", [P, KC, MT], BF16)
        wr = [sb("wr%d" % i, [P, KC, 256], BF16) for i in range(2)]
        qT = sb("qT", [P, 8, MT], BF16)
        kT = sb("kT", [P, 4, MT], BF16)
        vT = sb("vT", [P, 2, MT], BF16)
        uT = sb("uT", [P, 8, MT], BF16)
        raw = [sb("raw%d" % i, [P, MT], BF16) for i in range(2)]
        posi = sb("posi", [P, MT], I32)
        ang = sb("ang", [P, MT]); tq1 = sb("tq1", [P, MT]); tqi = posi
        tm = sb("tm", [P, MT])
        Ct = sb("Ct", [P, MT]); St = sb("St", [P, MT])
        t1 = tq1; t2 = tm
        kprev = sb("kprev", [P, 2, 4, P], BF16)
        kz = sb("kz", [P, 2, 4, MT], BF16)
        E("vector", lambda e: e.memset(kprev[:], 0.0), [], ["kprev"])
        v_tm = sb("v_tm", [P, 5, 4, 65], BF16)
        E("vector", lambda e: e.memset(v_tm[:], 1.0), [], ["v_tm"])
        pebuf = [sb("pe%d" % i, [P, 4 * P], BF16) for i in range(4)]
        den = sb("den", [P, 4])
        attn_tm = sb("attn_tm", [P, 1024], BF16)
        aoT = sb("aoT", [P, 8, MT], BF16)
        acnt = [0]
        wcount = [0]

        def rms_stats(b, src_key):
            E("scalar", lambda e, b=b: e.activation(out=xnb[b][:], in_=xb[b][:], func=AF.Square, accum_out=ss[b][:]),
              [src_key], ["xnb%d" % b, "ss%d" % b])
            E("scalar", lambda e, b=b: e.activation(out=rstd[b][:], in_=ss[b][:], func=AF.Sqrt,
                                                    scale=1.0 / D_MODEL, bias=RMS_EPS),
              ["ss%d" % b], ["rstd%d" % b])
            E("vector", lambda e, b=b: e.reciprocal(out=rstd[b][:], in_=rstd[b][:]),
              ["rstd%d" % b], ["rstd%d" % b])

        def reduce_angle(src_ap_fn, dst, shift):
            E("vector", lambda e: e.tensor_scalar(out=tq1[:], in0=ang[:], scalar1=shift, scalar2=1.0 / TWO_PI,
                                                  op0=ALU.add, op1=ALU.mult), ["ang"], ["tq1"])
            E("vector", lambda e: e.tensor_copy(out=tqi[:], in_=tq1[:]), ["tq1"], ["posi"])
            E("vector", lambda e: e.tensor_copy(out=tq1[:], in_=tqi[:]), ["posi"], ["tq1"])
            E("vector", lambda e: e.tensor_scalar(out=tm[:], in0=ang[:], scalar1=shift, scalar2=None,
                                                  op0=ALU.add), ["ang"], ["tm"])
            E("vector", lambda e: e.scalar_tensor_tensor(out=tm[:], in0=tq1[:], scalar=-TWO_PI, in1=tm[:],
                                                         op0=ALU.mult, op1=ALU.add), ["tq1", "tm"], ["tm"])
            E("vector", lambda e: e.tensor_scalar(out=tq1[:], in0=tm[:], scalar1=math.pi, scalar2=-TWO_PI,
                                                  op0=ALU.is_gt, op1=ALU.mult), ["tm"], ["tq1"])
            E("vector", lambda e: e.tensor_tensor(out=tm[:], in0=tm[:], in1=tq1[:], op=ALU.add), ["tm", "tq1"], ["tm"])
            E("vector", lambda e: e.tensor_scalar(out=tq1[:], in0=tm[:], scalar1=-math.pi, scalar2=TWO_PI,
                                                  op0=ALU.is_lt, op1=ALU.mult), ["tm"], ["tq1"])
            E("vector", lambda e: e.tensor_tensor(out=tm[:], in0=tm[:], in1=tq1[:], op=ALU.add), ["tm", "tq1"], ["tm"])
            E("scalar", lambda e: e.activation(out=dst[:], in_=tm[:], func=AF.Sin), ["tm"], [dst_key(dst)])

        keymap = {}

        def dst_key(t):
            return keymap[id(t)]
        keymap[id(Ct)] = "Ct"; keymap[id(St)] = "St"

        def load_w(cols):
            b = wcount[0] % 2
            wcount[0] += 1
            for (d0, s0, n) in cols:
                D("sync", wa[b][:, :, d0:d0 + n], w_in_v[:, :, s0:s0 + n], writes=["wa%d" % b])
            E("gpsimd", lambda e, b=b: e.tensor_copy(out=wr[b][:], in_=wa[b][:]), ["wa%d" % b], ["wr%d" % b])
            return b

        pcount = [0]

        def proj_chunk(wb, wc0, bias_col, dst_ap, dst_key_, rope):
            bank = 1 + (pcount[0] % 3)
            pcount[0] += 1
            for kc in range(KC):
                E("tensor", lambda e, kc=kc, bank=bank: e.matmul(
                    psb[bank][:, :], lhsT=wr[wb][:, kc, wc0:wc0 + 128], rhs=hT[:, kc, :],
                    start=(kc == 0), stop=(kc == KC - 1)), ["wr%d" % wb, "hT"], [psk[bank]])
            if not rope:
                E("scalar", lambda e, bank=bank: e.activation(out=dst_ap, in_=psb[bank][:, :], func=AF.Identity,
                                                              bias=binc[:, bias_col:bias_col + 1], scale=1.0),
                  [psk[bank], "binc"], [dst_key_])
                return
            rb = pcount[0] % 2
            E("scalar", lambda e, bank=bank, rb=rb: e.activation(out=raw[rb][:], in_=psb[bank][:, :], func=AF.Identity,
                                                                 bias=binc[:, bias_col:bias_col + 1], scale=1.0),
              [psk[bank], "binc"], ["raw%d" % rb])
            pb = 4 + (pcount[0] % 2)
            E("tensor", lambda e, pb=pb, rb=rb: e.matmul(psb[pb][:, :], lhsT=pm_b[:], rhs=raw[rb][:],
                                                         start=True, stop=True), ["pm_b", "raw%d" % rb], [psk[pb]])
            E("vector", lambda e, rb=rb: e.tensor_tensor(out=t1[:], in0=raw[rb][:], in1=Ct[:], op=ALU.mult),
              ["raw%d" % rb, "Ct"], ["tq1"])
            E("vector", lambda e, pb=pb: e.tensor_tensor(out=t2[:], in0=psb[pb][:, :], in1=St[:], op=ALU.mult),
              [psk[pb], "St"], ["tm"])
            E("vector", lambda e: e.tensor_tensor(out=dst_ap, in0=t1[:], in1=t2[:], op=ALU.add),
              ["tq1", "tm"], [dst_key_])


        SSM_ON = upto >= 5
        SSM_RUN = upto >= 6
        if SSM_ON:
            w_glu_v = w_glu.rearrange("(kc p) n -> p kc n", p=P)
            are = sb("s_are", [P, 32]); aim = sb("s_aim", [P, 32]); ldt = sb("s_ldt", [P, 32])
            D("sync", are[:], ssm_are_d, writes=["s_are"]); D("sync", aim[:], ssm_aim_d, writes=["s_aim"])
            D("sync", ldt[:], ssm_ldt_d, writes=["s_ldt"])
            dcol = sb("s_dcol", [P, 8]); D("sync", dcol[:], ssm_d_col_d, writes=["s_dcol"])
            bglu = sb("s_bglu", [P, 8]); D("sync", bglu[:], b_glu_col_d, writes=["s_bglu"])
            sv = {}; svk = {}
            def S(name):
                if name not in sv:
                    i = len(sv)
                    base = Ct if i < 16 else St
                    sv[name] = base[:, (i % 16) * 32:(i % 16 + 1) * 32]
                    svk[name] = "Ct" if i < 16 else "St"
                return sv[name]
            def SK(name):
                S(name)
                return svk[name]
            def V(op, o, a, b_=None, **kw):
                if b_ is None:
                    E("vector", lambda e, o=o, a=a, kw=kw: e.tensor_scalar(out=S(o), in0=S(a), **kw), [SK(a)], [SK(o)])
                else:
                    E("vector", lambda e, o=o, a=a, b_=b_, op=op: e.tensor_tensor(out=S(o), in0=S(a), in1=S(b_), op=op),
                      [SK(a), SK(b_)], [SK(o)])
            sv["are"], sv["aim"], sv["ldt"] = are[:], aim[:], ldt[:]
            svk["are"], svk["aim"], svk["ldt"] = "s_are", "s_aim", "s_ldt"
            E("scalar", lambda e: e.activation(out=S("dt"), in_=ldt[:], func=AF.Exp), ["s_ldt"], [SK("dt")])
            V(None, "lre", "are", scalar1=-1e-4, scalar2=None, op0=ALU.min)
            V(ALU.mult, "lrdt", "lre", "dt")
            E("scalar", lambda e: e.activation(out=S("mag"), in_=S("lrdt"), func=AF.Exp), [SK("lrdt")], [SK("mag")])
            V(ALU.mult, "th", "aim", "dt")
            sv["qi"] = posi[:, 0:32]; svk["qi"] = "posi"
            def sincos(dst, shift):
                V(None, "q1", "th", scalar1=shift, scalar2=1.0 / TWO_PI, op0=ALU.add, op1=ALU.mult)
                qi = sv["qi"]
                E("vector", lambda e: e.tensor_copy(out=qi, in_=S("q1")), [SK("q1")], ["posi"])
                E("vector", lambda e: e.tensor_copy(out=S("q1"), in_=qi), ["posi"], [SK("q1")])
                V(None, "r", "th", scalar1=shift, scalar2=None, op0=ALU.add)
                E("vector", lambda e: e.scalar_tensor_tensor(out=S("r"), in0=S("q1"), scalar=-TWO_PI, in1=S("r"),
                                                             op0=ALU.mult, op1=ALU.add), [SK("q1"), SK("r")], [SK("r")])
                V(None, "q1", "r", scalar1=math.pi, scalar2=-TWO_PI, op0=ALU.is_gt, op1=ALU.mult)
                V(ALU.add, "r", "r", "q1")
                V(None, "q1", "r", scalar1=-math.pi, scalar2=TWO_PI, op0=ALU.is_lt, op1=ALU.mult)
                V(ALU.add, "r", "r", "q1")
                E("scalar", lambda e: e.activation(out=S(dst)[:], in_=S("r"), func=AF.Sin), [SK("r")], [SK(dst)])
            sincos("sn", 0.0)
            sincos("cs", math.pi / 2.0)
            V(ALU.mult, "abr", "mag", "cs")
            V(ALU.mult, "abi", "mag", "sn")
            V(ALU.mult, "t_a", "lre", "lre"); V(ALU.mult, "t_b", "aim", "aim"); V(ALU.add, "den", "t_a", "t_b")
            E("vector", lambda e: e.reciprocal(out=S("rden"), in_=S("den")), [SK("den")], [SK("rden")])
            V(None, "nre", "abr", scalar1=-1.0, scalar2=None, op0=ALU.add)
            V(ALU.mult, "t_a", "nre", "lre"); V(ALU.mult, "t_b", "abi", "aim"); V(ALU.add, "zre", "t_a", "t_b")
            V(ALU.mult, "zre", "zre", "rden")
            V(ALU.mult, "t_a", "abi", "lre"); V(ALU.mult, "t_b", "nre", "aim"); V(ALU.subtract, "zim", "t_a", "t_b")
            V(ALU.mult, "zim", "zim", "rden")
            V(None, "nzim", "zim", scalar1=-1.0, scalar2=None, op0=ALU.mult)
            V(None, "nabi", "abi", scalar1=-1.0, scalar2=None, op0=ALU.mult)
            if debug:
                dab = sb("s_dab", [P, 64])
                E("vector", lambda e: e.tensor_copy(out=dab[:, 0:32], in_=S("abr")), [SK("abr")], ["s_dab"])
                E("vector", lambda e: e.tensor_copy(out=dab[:, 32:64], in_=S("abi")), [SK("abi")], ["s_dab"])
                D("sync", dbg["d_abar"], dab[:], reads=["s_dab"], is_output=True)
            hTf = hT[:].bitcast(F32)
            def scr(i):
                return hTf[:, 2 * i:2 * i + 2, :].rearrange("p a (b c) -> p (a b) c", c=16)
            bre, bim = scr(0), scr(1)
            D("sync", bre[:], ssm_bre_d, writes=["hT"]); D("sync", bim[:], ssm_bim_d, writes=["hT"])
            bbr, bbi, btmp = scr(2), scr(3), scr(4)
            def bc(n):
                return S(n).unsqueeze(2).broadcast_to([P, 32, 16])
            E("vector", lambda e: e.tensor_tensor(out=bbr[:], in0=bre[:], in1=bc("zre"), op=ALU.mult), ["hT", SK("zre")], ["hT"])
            E("vector", lambda e: e.tensor_tensor(out=btmp[:], in0=bim[:], in1=bc("nzim"), op=ALU.mult), ["hT", SK("nzim")], ["hT"])
            E("vector", lambda e: e.tensor_tensor(out=bbr[:], in0=bbr[:], in1=btmp[:], op=ALU.add), ["hT", "hT"], ["hT"])
            E("vector", lambda e: e.tensor_tensor(out=bbi[:], in0=bim[:], in1=bc("zre"), op=ALU.mult), ["hT", SK("zre")], ["hT"])
            E("vector", lambda e: e.tensor_tensor(out=btmp[:], in0=bre[:], in1=bc("zim"), op=ALU.mult), ["hT", SK("zim")], ["hT"])
            E("vector", lambda e: e.tensor_tensor(out=bbi[:], in0=bbi[:], in1=btmp[:], op=ALU.add), ["hT", "hT"], ["hT"])
            LB = sb("s_LB", [P, 64, P], BF16)
            bsrc = sb("s_bsrc", [P, P])
            for k in range(32):
                for ri, bb in ((0, bbr), (1, bbi)):
                    E("vector", lambda e: e.memset(bsrc[:], 0.0), [], ["s_bsrc"])
                    for g2 in range(2):
                        c0 = (k % 4) * 32 + g2 * 16
                        E("vector", lambda e, bb=bb, k=k, g2=g2, c0=c0: e.tensor_scalar(
                            out=bsrc[:, c0:c0 + 16], in0=bb[:, k, :], scalar1=rowmask[:, g2:g2 + 1], scalar2=None,
                            op0=ALU.mult), ["hT", "hT", "rowmask"], ["s_bsrc"])
                    E("tensor", lambda e: e.transpose(out=psb[0][:, 0:P], in_=bsrc[:], identity=ident_f[:]),
                      ["s_bsrc", "ident_f"], ["psb0"])
                    E("scalar", lambda e, k=k, ri=ri: e.activation(out=LB[:, 2 * k + ri, :], in_=psb[0][:, 0:P],
                                                                   func=AF.Identity), ["psb0"], ["s_LB"])
            cre, cim = scr(5), scr(6)
            D("sync", cre[:], ssm_cre_d, writes=["hT"]); D("sync", cim[:], ssm_cim_d, writes=["hT"])
            CP = sb("s_CP", [P, 64, P], BF16)
            E("vector", lambda e: e.memset(CP[:], 0.0), [], ["s_CP"])
            for k in range(32):
                for ri, cc, sgn in ((0, cre, 1.0), (1, cim, -1.0)):
                    for g2 in range(2):
                        c0 = (k % 4) * 32 + g2 * 16
                        E("vector", lambda e, cc=cc, k=k, ri=ri, g2=g2, c0=c0, sgn=sgn: e.tensor_scalar(
                            out=CP[:, 2 * k + ri, c0:c0 + 16], in0=cc[:, k, :], scalar1=rowmask[:, g2:g2 + 1],
                            scalar2=sgn, op0=ALU.mult, op1=ALU.mult), ["hT", "hT", "rowmask"], ["s_CP"])
            Aar = sb("s_Aar", [P, 2, 32]); Aai = sb("s_Aai", [P, 2, 32])
            for ri in range(2):
                E("vector", lambda e, ri=ri: e.tensor_copy(out=Aar[:, ri, :], in_=S("abr")), [SK("abr")], ["s_Aar"])
            E("vector", lambda e: e.tensor_copy(out=Aai[:, 0, :], in_=S("nabi")), [SK("nabi")], ["s_Aai"])
            E("vector", lambda e: e.tensor_copy(out=Aai[:, 1, :], in_=S("abi")), [SK("abi")], ["s_Aai"])
            LCH = 32
            BUs = xb[0][:].rearrange("p (r k t) -> p r k t", r=2, k=32)
            Xh = xb[1][:].rearrange("p (r k t) -> p r k t", r=2, k=32)
            Xb = sb("s_Xb", [P, 2, 32, LCH], BF16)
            Xst = sb("s_Xst", [P, 2, 32])
            E("vector", lambda e: e.memset(Xst[:], 0.0), [], ["s_Xst"])
            T1 = sb("s_T1", [P, 2, 32]); T2 = sb("s_T2", [P, 2, 32])
            yT = qT
            zT = kz[:].rearrange("p a g t -> p (a g) t")
            soT = sb("s_soT", [P, 8, MT], BF16)

        PEER_ON = upto >= 8
        if PEER_ON:
            w_query_v = w_query.rearrange("(kc p) n -> p kc n", p=P)
            gffn = sb("gffn", [P, KC]); D("sync", gffn[:], g_ffn_col_d, writes=["gffn"])
            A2 = sb("A2", [P, KC])
            E("vector", lambda e: e.scalar_tensor_tensor(out=A2[:], in0=modT[:, 64:80], scalar=1.0, in1=gffn[:],
                                                         op0=ALU.add, op1=ALU.mult), ["modT", "gffn"], ["A2"])
            iota16 = sb("iota16", [P, 16]); D("sync", iota16[:], iota16_d, writes=["iota16"])
            skT = sb("skT", [P, 16, P], BF16)
            D("sync", wa[0][:, :, 0:P], sk_l_d, writes=["wa0"])
            E("vector", lambda e: e.tensor_copy(out=skT[:], in_=wa[0][:, :, 0:P]), ["wa0"], ["skT"])
        n_macro = (1 if upto >= 1 else 0) if debug else NM
        for m in range(n_macro):
            for tl in range(4):
                t = m * 4 + tl
                b = t % 2
                D("sync", xb[b][:], x[t * P:(t + 1) * P, :], writes=["xb%d" % b])
                rms_stats(b, "xb%d" % b)
                E("vector", lambda e, b=b: e.tensor_scalar(out=xnb[b][:], in0=xb[b][:], scalar1=rstd[b][:],
                                                           scalar2=None, op0=ALU.mult),
                  ["xb%d" % b, "rstd%d" % b], ["xnb%d" % b])
                for kc in range(KC):
                    E("tensor", lambda e, b=b, kc=kc: e.transpose(
                        out=psT[:, kc * P:(kc + 1) * P], in_=xnb[b][:, kc * P:(kc + 1) * P], identity=ident_b[:]),
                        ["xnb%d" % b, "ident_b"], ["psT"])
                for kc in range(KC):
                    E("vector", lambda e, kc=kc, tl=tl: e.tensor_scalar(
                        out=hT[:, kc, tl * P:(tl + 1) * P], in0=psT[:, kc * P:(kc + 1) * P],
                        scalar1=A1[:, kc:kc + 1], scalar2=modT[:, kc:kc + 1], op0=ALU.mult, op1=ALU.add),
                        ["psT", "A1", "modT"], ["hT"])
            if upto < 2:
                if debug:
                    D('sync', dbg['d_hT'], hT[:, 0:8, :], reads=['hT'], is_output=True)
                continue
            D("sync", posi[:], pos[:, m * MT:(m + 1) * MT].partition_broadcast(P), writes=["posi"])
            E("vector", lambda e: e.tensor_copy(out=ang[:], in_=posi[:]), ["posi"], ["ang"])
            E("vector", lambda e: e.tensor_scalar(out=ang[:], in0=ang[:], scalar1=invf[:, 0:1], scalar2=None,
                                                  op0=ALU.mult), ["ang", "invf"], ["ang"])
            reduce_angle(None, St, 0.0)
            reduce_angle(None, Ct, math.pi / 2.0)
            if upto < 3:
                continue
            for blk in range(4):
                wb = load_w([(0, blk * 256, 256)])
                for sub in range(2):
                    j = blk * 2 + sub
                    proj_chunk(wb, sub * 128, j, qT[:, j, :], "qT", True)
            for gp in range(2):
                g0, g1 = 2 * gp, 2 * gp + 1
                wb = load_w([(0, 1024 + g0 * 64, 64), (64, 1024 + g0 * 64, 64),
                             (128, 1024 + g1 * 64, 64), (192, 1024 + g1 * 64, 64)])
                proj_chunk(wb, 0, 52 + g0, kT[:, g0, :], "kT", True)
                proj_chunk(wb, 128, 52 + g1, kT[:, g1, :], "kT", True)
            wb = load_w([(0, 1280, 256)])
            for sub in range(2):
                proj_chunk(wb, sub * 128, 10 + sub, vT[:, sub, :], "vT", False)
            for blk in range(4):
                wb = load_w([(0, 1536 + blk * 256, 256)])
                for sub in range(2):
                    j = blk * 2 + sub
                    proj_chunk(wb, sub * 128, 12 + j, uT[:, j, :], "uT", False)
            if upto >= 4:
                for hl in range(2):
                    E("vector", lambda e, hl=hl: e.tensor_scalar(
                        out=kz[:, hl, :, :], in0=kT[:, :, :], scalar1=rowmask[:, hl:hl + 1], scalar2=None,
                        op0=ALU.mult), ["kT", "rowmask"], ["kz"])
                for blk in range(4):
                    for c2 in range(2):
                        E("tensor", lambda e, blk=blk, c2=c2: e.transpose(
                            out=psT[:, c2 * P:(c2 + 1) * P], in_=vT[:, c2, blk * P:(blk + 1) * P], identity=ident_b[:]),
                            ["vT", "ident_b"], ["psT"])
                    E("vector", lambda e, blk=blk: e.tensor_copy(
                        out=v_tm[:, 1 + blk, :, 0:64], in_=psT[:, 0:256].rearrange("p (g d) -> p g d", d=64)),
                        ["psT"], ["v_tm"])
                ATT_SUB = int(os.environ.get("ATT_SUB", "9"))
                for b in range(4 if ATT_SUB >= 2 else 0):
                    gb = 4 * m + b
                    for g in range(4):
                        kbs = (["prev"] if gb > 0 else []) + ["cur"]
                        pes = []
                        for kb in kbs:
                            bank = 1 + (acnt[0] % 3)
                            pi = acnt[0] % 4
                            acnt[0] += 1
                            for i in range(4):
                                hq = 4 * g + i
                                chunk = hq // 2
                                base = (hq % 2) * 64
                                hl = hq % 2
                                if kb == "cur":
                                    ksrc = kz[:, hl, g, b * P:(b + 1) * P]; kkey = "kz"
                                elif b > 0:
                                    ksrc = kz[:, hl, g, (b - 1) * P:b * P]; kkey = "kz"
                                else:
                                    ksrc = kprev[:, hl, g, :]; kkey = "kprev"
                                E("tensor", lambda e, bank=bank, i=i, ksrc=ksrc, chunk=chunk, b=b: e.matmul(
                                    psb[bank][:, i * P:(i + 1) * P], lhsT=ksrc,
                                    rhs=qT[:, chunk, b * P:(b + 1) * P], start=True, stop=True),
                                    [kkey, "qT"], [psk[bank]])
                            pe = pebuf[pi]
                            E("scalar", lambda e, pe=pe, bank=bank: e.activation(
                                out=pe[:], in_=psb[bank][:, :], func=AF.Exp, scale=0.125), [psk[bank]], ["pe%d" % pi])
                            mk = mcur if kb == "cur" else mprev
                            mkey = "mcur" if kb == "cur" else "mprev"
                            E("vector", lambda e, pe=pe, mk=mk: e.tensor_tensor(
                                out=pe[:].rearrange("p (h q) -> p h q", q=P), in0=pe[:].rearrange("p (h q) -> p h q", q=P),
                                in1=mk[:].unsqueeze(1).broadcast_to([P, 4, P]), op=ALU.mult),
                                ["pe%d" % pi, mkey], ["pe%d" % pi])
                            slot = (1 + b) if kb == "cur" else b
                            pes.append((pe, "pe%d" % pi, slot))
                        pob = 4 + (acnt[0] % 2)
                        if ATT_SUB < 3:
                            continue
                        for i in range(4):
                            for n, (pe, pkey, slot) in enumerate(pes):
                                E("tensor", lambda e, pob=pob, i=i, pe=pe, slot=slot, g=g, n=n, L=len(pes): e.matmul(
                                    psb[pob][:, i * 65:(i + 1) * 65], lhsT=pe[:, i * P:(i + 1) * P],
                                    rhs=v_tm[:, slot, g, :], start=(n == 0), stop=(n == L - 1)),
                                    [pkey, "v_tm"], [psk[pob]])
                        if ATT_SUB < 4:
                            continue
                        po3 = psb[pob][:, 0:260].rearrange("p (h d) -> p h d", d=65)
                        E("vector", lambda e, po3=po3, g=g: e.tensor_tensor(
                            out=den[:], in0=po3[:, :, 64], in1=esink[:, 4 * g:4 * g + 4], op=ALU.add),
                            [psk[pob], "esink"], ["den"])
                        E("vector", lambda e: e.reciprocal(out=den[:], in_=den[:]), ["den"], ["den"])
                        E("vector", lambda e, po3=po3, g=g: e.tensor_tensor(
                            out=attn_tm[:, 4 * g * 64:(4 * g + 4) * 64].rearrange("p (h d) -> p h d", d=64),
                            in0=po3[:, :, 0:64], in1=den[:].unsqueeze(2).broadcast_to([P, 4, 64]), op=ALU.mult),
                            [psk[pob], "den"], ["attn_tm"])
                    for c in range(8 if ATT_SUB >= 5 else 0):
                        E("tensor", lambda e, c=c: e.transpose(
                            out=psT[:, c * P:(c + 1) * P], in_=attn_tm[:, c * P:(c + 1) * P], identity=ident_b[:]),
                            ["attn_tm", "ident_b"], ["psT"])
                    E("vector", lambda e, b=b: e.tensor_copy(
                        out=aoT[:, :, b * P:(b + 1) * P], in_=psT[:, 0:1024].rearrange("p (c t) -> p c t", t=P)),
                        ["psT"], ["aoT"])
                for hl in range(2):
                    E("vector", lambda e, hl=hl: e.tensor_copy(out=kprev[:, hl, :, :], in_=kz[:, hl, :, 3 * P:4 * P]),
                      ["kz"], ["kprev"])
                E("vector", lambda e: e.tensor_copy(out=v_tm[:, 0, :, :], in_=v_tm[:, 4, :, :]), ["v_tm"], ["v_tm"])
                if debug and m == 0:
                    D("sync", dbg["d_att"], aoT[:], reads=["aoT"], is_output=True)
            if debug and m == 0:
                for nm, src, key, n in (("d_q", qT, "qT", 8), ("d_k", kT, "kT", 4),
                                        ("d_v", vT, "vT", 2), ("d_u", uT, "uT", 8)):
                    D("sync", dbg[nm], src[:], reads=[key], is_output=True)

            if SSM_ON and SSM_RUN:
                NCH = MT // LCH
                for c in range(NCH):
                    tc0 = c * LCH
                    for ri in range(2):
                        for half in range(2):
                            bank = 1 + 2 * ri + half
                            for kk in range(16):
                                k = half * 16 + kk
                                E("tensor", lambda e, bank=bank, kk=kk, k=k, ri=ri, tc0=tc0: e.matmul(
                                    psb[bank][:, kk * LCH:(kk + 1) * LCH], lhsT=LB[:, 2 * k + ri, :],
                                    rhs=uT[:, k // 4, tc0:tc0 + LCH], start=True, stop=True),
                                    ["s_LB", "uT"], [psk[bank]])
                            E("scalar", lambda e, bank=bank, ri=ri, half=half: e.activation(
                                out=BUs[:, ri, half * 16:(half + 1) * 16, :],
                                in_=psb[bank][:, :].rearrange("p (k t) -> p k t", t=LCH), func=AF.Identity),
                                [psk[bank]], ["xb0"])
                    for sidx in range(LCH):
                        if c == 0 and sidx == 0:
                            prev = Xst; pkey = "s_Xst"
                            pv = lambda r_: Xst[:, r_, :]
                            pall = Xst[:]
                        else:
                            col = (sidx - 1) % LCH
                            pkey = "xb1"
                            pv = lambda r_, col=col: Xh[:, r_, :, col]
                            pall = Xh[:, :, :, col]
                        E("vector", lambda e, pall=pall: e.tensor_tensor(out=T1[:], in0=Aar[:], in1=pall, op=ALU.mult),
                          ["s_Aar", pkey], ["s_T1"])
                        E("vector", lambda e, pv=pv: e.tensor_tensor(out=T2[:, 0, :], in0=Aai[:, 0, :], in1=pv(1), op=ALU.mult),
                          ["s_Aai", pkey], ["s_T2"])
                        E("vector", lambda e, pv=pv: e.tensor_tensor(out=T2[:, 1, :], in0=Aai[:, 1, :], in1=pv(0), op=ALU.mult),
                          ["s_Aai", pkey], ["s_T2"])
                        E("vector", lambda e: e.tensor_tensor(out=T1[:], in0=T1[:], in1=T2[:], op=ALU.add),
                          ["s_T1", "s_T2"], ["s_T1"])
                        E("vector", lambda e, sidx=sidx: e.tensor_tensor(out=Xh[:, :, :, sidx], in0=T1[:], in1=BUs[:, :, :, sidx],
                                                                         op=ALU.add), ["s_T1", "xb0"], ["xb1"])
                    for ri in range(2):
                        E("gpsimd", lambda e, ri=ri: e.tensor_copy(out=Xb[:, ri, :, :], in_=Xh[:, ri, :, :]), ["xb1"], ["s_Xb"])
                    ybank = 5
                    for kt in range(8):
                        n = 0
                        for kk in range(4):
                            k = 4 * kt + kk
                            for ri in range(2):
                                E("tensor", lambda e, kt=kt, k=k, ri=ri, n=n: e.matmul(
                                    psb[ybank][:, kt * LCH:(kt + 1) * LCH], lhsT=CP[:, 2 * k + ri, :],
                                    rhs=Xb[:, ri, k, :], start=(n == 0), stop=(n == 7)), ["s_CP", "s_Xb"], [psk[ybank]])
                                n += 1
                    for kt in range(8):
                        E("vector", lambda e, kt=kt, tc0=tc0: e.scalar_tensor_tensor(
                            out=yT[:, kt, tc0:tc0 + LCH], in0=uT[:, kt, tc0:tc0 + LCH], scalar=dcol[:, kt:kt + 1],
                            in1=psb[ybank][:, kt * LCH:(kt + 1) * LCH], op0=ALU.mult, op1=ALU.add),
                            ["uT", "s_dcol", psk[ybank]], ["qT"])
                E("vector", lambda e: e.tensor_copy(out=Xst[:], in_=Xh[:, :, :, LCH - 1]), ["xb1"], ["s_Xst"])
                if debug and m == 0:
                    D("sync", dbg["d_y"], yT[:], reads=["qT"], is_output=True)
                for kt in range(8):
                    xk = yT[:, kt, :]
                    E("vector", lambda e, xk=xk: e.tensor_tensor(out=tq1[:], in0=xk, in1=xk, op=ALU.mult), ["qT"], ["tq1"])
                    E("vector", lambda e: e.tensor_scalar(out=tq1[:], in0=tq1[:], scalar1=0.044715, scalar2=1.0,
                                                          op0=ALU.mult, op1=ALU.add), ["tq1"], ["tq1"])
                    E("vector", lambda e, xk=xk: e.tensor_tensor(out=tq1[:], in0=tq1[:], in1=xk, op=ALU.mult), ["tq1", "qT"], ["tq1"])
                    E("scalar", lambda e: e.activation(out=tm[:], in_=tq1[:], func=AF.Tanh, scale=math.sqrt(2.0 / math.pi)),
                      ["tq1"], ["tm"])
                    E("vector", lambda e, xk=xk: e.scalar_tensor_tensor(out=tm[:], in0=tm[:], scalar=1.0, in1=xk,
                                                                        op0=ALU.add, op1=ALU.mult), ["tm", "qT"], ["tm"])
                    E("scalar", lambda e, kt=kt: e.activation(out=zT[:, kt, :], in_=tm[:], func=AF.Identity, scale=0.5),
                      ["tm"], ["kz"])
                for blk in range(4):
                    b = wcount[0] % 2
                    wcount[0] += 1
                    D("sync", wa[b][:, 0:8, :], w_glu_v[:, :, blk * 256:(blk + 1) * 256], writes=["wa%d" % b])
                    E("gpsimd", lambda e, b=b: e.tensor_copy(out=wr[b][:, 0:8, :], in_=wa[b][:, 0:8, :]), ["wa%d" % b], ["wr%d" % b])
                    for sub in range(2):
                        j = blk * 2 + sub
                        bank = 1 + (pcount[0] % 3)
                        pcount[0] += 1
                        for kc in range(8):
                            E("tensor", lambda e, kc=kc, bank=bank, b=b, sub=sub: e.matmul(
                                psb[bank][:, :], lhsT=wr[b][:, kc, sub * P:(sub + 1) * P], rhs=zT[:, kc, :],
                                start=(kc == 0), stop=(kc == 7)), ["wr%d" % b, "kz"], [psk[bank]])
                        E("scalar", lambda e, bank=bank, j=j: e.activation(out=tq1[:], in_=psb[bank][:, :], func=AF.Sigmoid,
                                                                           bias=bglu[:, j:j + 1], scale=1.0),
                          [psk[bank], "s_bglu"], ["tq1"])
                        E("vector", lambda e, j=j: e.tensor_tensor(out=soT[:, j, :], in0=zT[:, j, :], in1=tq1[:], op=ALU.mult),
                          ["kz", "tq1"], ["s_soT"])
                if debug and m == 0:
                    D("sync", dbg["d_ssm"], soT[:], reads=["s_soT"], is_output=True)

            if upto >= 7:
                w_ab_v = w_ab.rearrange("(kc p) n -> p kc n", p=P)
                w_sb_v = w_sb.rearrange("(kc p) n -> p kc n", p=P)
                w_out_v = w_out.rearrange("(kc p) n -> p kc n", p=P)
                mg = [qT, kz[:].rearrange("p a g t -> p (a g) t")]
                mgk = ["qT", "kz"]
                for j in range(16):
                    ba = wcount[0] % 2; wcount[0] += 1
                    D("sync", wa[ba][:, 0:8, 0:P], w_ab_v[:, :, j * P:(j + 1) * P], writes=["wa%d" % ba])
                    D("sync", wa[ba][:, 0:8, P:2 * P], w_sb_v[:, :, j * P:(j + 1) * P], writes=["wa%d" % ba])
                    E("gpsimd", lambda e, ba=ba: e.tensor_copy(out=wr[ba][:, 0:8, :], in_=wa[ba][:, 0:8, :]),
                      ["wa%d" % ba], ["wr%d" % ba])
                    for kc in range(8):
                        E("tensor", lambda e, kc=kc, ba=ba: e.matmul(psb[1][:, :], lhsT=wr[ba][:, kc, 0:P], rhs=aoT[:, kc, :],
                                                                     start=(kc == 0), stop=(kc == 7)), ["wr%d" % ba, "aoT"], ["psb1"])
                    for kc in range(8):
                        E("tensor", lambda e, kc=kc, ba=ba: e.matmul(psb[2][:, :], lhsT=wr[ba][:, kc, P:2 * P], rhs=soT[:, kc, :],
                                                                     start=(kc == 0), stop=(kc == 7)), ["wr%d" % ba, "s_soT"], ["psb2"])
                    bg = wcount[0] % 2; wcount[0] += 1
                    D("sync", wa[bg][:, :, 0:P], w_in_v[:, :, 2560 + j * P:2560 + (j + 1) * P], writes=["wa%d" % bg])
                    D("sync", wa[bg][:, :, P:2 * P], w_in_v[:, :, 4608 + j * P:4608 + (j + 1) * P], writes=["wa%d" % bg])
                    E("gpsimd", lambda e, bg=bg: e.tensor_copy(out=wr[bg][:], in_=wa[bg][:]), ["wa%d" % bg], ["wr%d" % bg])
                    for kc in range(KC):
                        E("tensor", lambda e, kc=kc, bg=bg: e.matmul(psb[3][:, :], lhsT=wr[bg][:, kc, 0:P], rhs=hT[:, kc, :],
                                                                     start=(kc == 0), stop=(kc == KC - 1)), ["wr%d" % bg, "hT"], ["psb3"])
                    for kc in range(KC):
                        E("tensor", lambda e, kc=kc, bg=bg: e.matmul(psb[4][:, :], lhsT=wr[bg][:, kc, P:2 * P], rhs=hT[:, kc, :],
                                                                     start=(kc == 0), stop=(kc == KC - 1)), ["wr%d" % bg, "hT"], ["psb4"])
                    E("scalar", lambda e, j=j: e.activation(out=tq1[:], in_=psb[3][:, :], func=AF.Sigmoid,
                                                            bias=binc[:, 20 + j:21 + j], scale=1.0), ["psb3", "binc"], ["tq1"])
                    E("scalar", lambda e, j=j: e.activation(out=tm[:], in_=psb[4][:, :], func=AF.Sigmoid,
                                                            bias=binc[:, 36 + j:37 + j], scale=1.0), ["psb4", "binc"], ["tm"])
                    E("vector", lambda e: e.tensor_tensor(out=tq1[:], in0=tq1[:], in1=psb[1][:, :], op=ALU.mult), ["tq1", "psb1"], ["tq1"])
                    E("vector", lambda e: e.tensor_tensor(out=tm[:], in0=tm[:], in1=psb[2][:, :], op=ALU.mult), ["tm", "psb2"], ["tm"])
                    E("vector", lambda e, j=j: e.tensor_tensor(out=mg[j // 8][:, j % 8, :], in0=tq1[:], in1=tm[:], op=ALU.add),
                      ["tq1", "tm"], [mgk[j // 8]])
                if debug and m == 0:
                    D("sync", dbg["d_mrg"], mg[0][:], reads=["qT"], is_output=True)
                g1b = xb[1]
                gfin_u = uT[:].bitcast(F32).rearrange("p a b -> p (a b)")
                for j in range(16):
                    E("vector", lambda e, j=j: e.tensor_scalar(out=mtmp[:], in0=ident_f[:], scalar1=modT[:, 32 + j:33 + j],
                                                               scalar2=None, op0=ALU.mult), ["ident_f", "modT"], ["mtmp"])
                    E("tensor", lambda e, j=j: e.matmul(psb[5][:, (j % 4) * P:(j % 4 + 1) * P], lhsT=ones_f[:], rhs=mtmp[:],
                                                        start=True, stop=True), ["ones_f", "mtmp"], ["psb5"])
                    if j % 4 == 3:
                        E("scalar", lambda e, j=j: e.activation(out=g1b[:, (j - 3) * P:(j + 1) * P], in_=psb[5][:, :],
                                                                func=AF.Identity), ["psb5"], ["xb1"])
                for tt in range(4):
                    t = m * 4 + tt
                    D("sync", xb[0][:], x[t * P:(t + 1) * P, :], writes=["xb0"])
                    for blk in range(8):
                        bo = wcount[0] % 2; wcount[0] += 1
                        D("sync", wa[bo][:], w_out_v[:, :, blk * 256:(blk + 1) * 256], writes=["wa%d" % bo])
                        E("gpsimd", lambda e, bo=bo: e.tensor_copy(out=wr[bo][:], in_=wa[bo][:]), ["wa%d" % bo], ["wr%d" % bo])
                        bank = 1 + (pcount[0] % 3); pcount[0] += 1
                        for kc in range(KC):
                            E("tensor", lambda e, kc=kc, bo=bo, bank=bank, tt=tt: e.matmul(
                                psb[bank][:, 0:256], lhsT=mg[kc // 8][:, kc % 8, tt * P:(tt + 1) * P], rhs=wr[bo][:, kc, :],
                                start=(kc == 0), stop=(kc == KC - 1)), ["wr%d" % bo, "qT", "kz"], [psk[bank]])
                        cs = slice(blk * 256, (blk + 1) * 256)
                        E("vector", lambda e, bank=bank, cs=cs: e.tensor_tensor(out=tq1[:, 0:256], in0=psb[bank][:, 0:256],
                                                                               in1=g1b[:, cs], op=ALU.mult), [psk[bank], "xb1"], ["tq1"])
                        E("vector", lambda e, cs=cs: e.tensor_tensor(out=xb[0][:, cs], in0=xb[0][:, cs], in1=tq1[:, 0:256],
                                                                     op=ALU.add), ["xb0", "tq1"], ["xb0"])
                    if debug and m == 0 and tt == 0:
                        D("sync", dbg["d_x1"], xb[0][:], reads=["xb0"], is_output=True)
                    if PEER_ON:
                        h2T = hT[:, :, 0:P]; qpT = hT[:, :, P:2 * P]; h2tm = xnb[1]; junk = xnb[0]
                        sc = aoT[:].bitcast(F32).rearrange("p a (b c) -> p (a b) c", c=P)
                        sc2 = soT[:].bitcast(F32).rearrange("p a (b c) -> p (a b) c", c=P)
                        cand = soT[:].bitcast(F32)
                        vt = Ct[:, 0:256].rearrange("p (a b) -> p a b", b=16)
                        ctv = Ct[:, 256:384].rearrange("p (a b) -> p a b", b=16)
                        gt = Ct[:, 384:512].rearrange("p (a b) -> p a b", b=16)
                        itf = St[:, 0:256].rearrange("p (a b) -> p a b", b=16)
                        af = St[:, 256:384].rearrange("p (a b) -> p a b", b=16)
                        bf_ = St[:, 384:512].rearrange("p (a b) -> p a b", b=16)
                        ik = ang[:, 0:128].rearrange("p (a b) -> p a b", b=16)
                        jk = ang[:, 128:256].rearrange("p (a b) -> p a b", b=16)
                        ef = ang[:, 256:384]; acol = ang[:, 384:512]
                        pu = posi[:].bitcast(U32)
                        it = pu[:, 0:256].rearrange("p (a b) -> p a b", b=16)
                        ci = pu[:, 256:384].rearrange("p (a b) -> p a b", b=16)
                        eidx = pu[:, 384:512]
                        oh = tq1[:, 0:256].rearrange("p (a b) -> p a b", b=16)
                        wcol = tq1[:, 256:384]; gsm = tq1[:, 384:392]
                        gbuf = uT[:].bitcast(F32).rearrange("p a b -> p (a b)")
                        acc_lo = Xb[:].bitcast(F32).rearrange("p r k t -> p (r k t)")
                        acc_hi = kT[:].bitcast(F32).rearrange("p g t -> p (g t)")
                        rms_stats(0, "xb0")
                        E("vector", lambda e: e.tensor_scalar(out=xnb[0][:], in0=xb[0][:], scalar1=rstd[0][:], scalar2=None,
                                                              op0=ALU.mult), ["xb0", "rstd0"], ["xnb0"])
                        for kc in range(KC):
                            E("tensor", lambda e, kc=kc: e.transpose(out=psT[:, kc * P:(kc + 1) * P],
                                                                     in_=xnb[0][:, kc * P:(kc + 1) * P], identity=ident_b[:]),
                              ["xnb0", "ident_b"], ["psT"])
                        for kc in range(KC):
                            E("vector", lambda e, kc=kc: e.tensor_scalar(
                                out=h2T[:, kc, :], in0=psT[:, kc * P:(kc + 1) * P], scalar1=A2[:, kc:kc + 1],
                                scalar2=modT[:, 48 + kc:49 + kc], op0=ALU.mult, op1=ALU.add), ["psT", "A2", "modT"], ["hT"])
                        for kc in range(KC):
                            E("tensor", lambda e, kc=kc: e.transpose(out=psT[:, kc * P:(kc + 1) * P], in_=h2T[:, kc, :],
                                                                     identity=ident_b[:]), ["hT", "ident_b"], ["psT"])
                        E("vector", lambda e: e.tensor_copy(out=h2tm[:], in_=psT[:, :]), ["psT"], ["xnb1"])
                        for blk in range(8):
                            bq = wcount[0] % 2; wcount[0] += 1
                            D("sync", wa[bq][:], w_query_v[:, :, blk * 256:(blk + 1) * 256], writes=["wa%d" % bq])
                            E("gpsimd", lambda e, bq=bq: e.tensor_copy(out=wr[bq][:], in_=wa[bq][:]), ["wa%d" % bq], ["wr%d" % bq])
                            for sub in range(2):
                                hc = blk * 2 + sub
                                bank = 1 + (pcount[0] % 3); pcount[0] += 1
                                for kc in range(KC):
                                    E("tensor", lambda e, kc=kc, bq=bq, sub=sub, bank=bank: e.matmul(
                                        psb[bank][:, 0:P], lhsT=wr[bq][:, kc, sub * P:(sub + 1) * P], rhs=h2T[:, kc, :],
                                        start=(kc == 0), stop=(kc == KC - 1)), ["wr%d" % bq, "hT"], [psk[bank]])
                                E("scalar", lambda e, hc=hc, bank=bank: e.activation(out=qpT[:, hc, :], in_=psb[bank][:, 0:P],
                                                                                     func=AF.Identity), [psk[bank]], ["hT"])
                        for hc in range(16):
                            E("tensor", lambda e, hc=hc: e.matmul(psb[4][:, (hc % 4) * P:(hc % 4 + 1) * P], lhsT=qpT[:, hc, :],
                                                                  rhs=skT[:, hc, :], start=True, stop=True), ["hT", "skT"], ["psb4"])
                            if hc % 4 == 3:
                                E("scalar", lambda e, hc=hc: e.activation(
                                    out=sc[:, hc - 3:hc + 1, :], in_=psb[4][:, :].rearrange("p (a b) -> p a b", b=P),
                                    func=AF.Identity), ["psb4"], ["aoT"])
                        for hc in range(16):
                            E("vector", lambda e, hc=hc: e.max(out=vt[:, hc, 0:8], in_=sc[:, hc, :]), ["aoT"], ["Ct"])
                            E("vector", lambda e, hc=hc: e.max_index(out=it[:, hc, 0:8], in_max=vt[:, hc, 0:8], in_values=sc[:, hc, :]),
                              ["aoT", "Ct"], ["posi"])
                            E("vector", lambda e, hc=hc: e.match_replace(out=sc2[:, hc, :], in_to_replace=vt[:, hc, 0:8],
                                                                         in_values=sc[:, hc, :], imm_value=-1e30),
                              ["aoT", "Ct"], ["s_soT"])
                            E("vector", lambda e, hc=hc: e.max(out=vt[:, hc, 8:16], in_=sc2[:, hc, :]), ["s_soT"], ["Ct"])
                            E("vector", lambda e, hc=hc: e.max_index(out=it[:, hc, 8:16], in_max=vt[:, hc, 8:16], in_values=sc2[:, hc, :]),
                              ["s_soT", "Ct"], ["posi"])
                        E("vector", lambda e: e.tensor_copy(out=itf, in_=it), ["posi"], ["St"])
                        for h in range(8):
                            E("vector", lambda e, h=h: e.tensor_tensor(
                                out=cand[:, h, :].rearrange("p (a b) -> p a b", b=16),
                                in0=vt[:, 2 * h, :].unsqueeze(2).broadcast_to([P, 16, 16]),
                                in1=vt[:, 2 * h + 1, :].unsqueeze(1).broadcast_to([P, 16, 16]), op=ALU.add), ["Ct"], ["s_soT"])
                        for h in range(8):
                            E("vector", lambda e, h=h: e.max(out=ctv[:, h, 0:8], in_=cand[:, h, :]), ["s_soT"], ["Ct"])
                            E("vector", lambda e, h=h: e.max_index(out=ci[:, h, 0:8], in_max=ctv[:, h, 0:8], in_values=cand[:, h, :]),
                              ["s_soT", "Ct"], ["posi"])
                            E("vector", lambda e, h=h: e.match_replace(out=cand[:, h, :], in_to_replace=ctv[:, h, 0:8],
                                                                       in_values=cand[:, h, :], imm_value=-1e30),
                              ["s_soT", "Ct"], ["s_soT"])
                            E("vector", lambda e, h=h: e.max(out=ctv[:, h, 8:16], in_=cand[:, h, :]), ["s_soT"], ["Ct"])
                            E("vector", lambda e, h=h: e.max_index(out=ci[:, h, 8:16], in_max=ctv[:, h, 8:16], in_values=cand[:, h, :]),
                              ["s_soT", "Ct"], ["posi"])
                        E("vector", lambda e: e.tensor_tensor(out=gt, in0=ctv, in1=ctv[:, :, 0:1].broadcast_to([P, 8, 16]),
                                                              op=ALU.subtract), ["Ct"], ["Ct"])
                        E("scalar", lambda e: e.activation(out=gt, in_=gt, func=AF.Exp), ["Ct"], ["Ct"])
                        E("vector", lambda e: e.tensor_reduce(out=gsm, in_=gt, axis=mybir.AxisListType.X, op=ALU.add), ["Ct"], ["tq1"])
                        E("vector", lambda e: e.reciprocal(out=gsm, in_=gsm), ["tq1"], ["tq1"])
                        E("vector", lambda e: e.tensor_tensor(out=gt, in0=gt, in1=gsm.unsqueeze(2).broadcast_to([P, 8, 16]),
                                                              op=ALU.mult), ["Ct", "tq1"], ["Ct"])
                        E("vector", lambda e: e.tensor_single_scalar(out=eidx.rearrange("p (a b) -> p a b", b=16), in_=ci, scalar=4,
                                                                      op=ALU.logical_shift_right), ["posi"], ["posi"])
                        E("vector", lambda e: e.tensor_copy(out=af, in_=eidx.rearrange("p (a b) -> p a b", b=16)), ["posi"], ["St"])
                        E("vector", lambda e: e.tensor_single_scalar(out=eidx.rearrange("p (a b) -> p a b", b=16), in_=ci, scalar=15,
                                                                      op=ALU.bitwise_and), ["posi"], ["posi"])
                        E("vector", lambda e: e.tensor_copy(out=bf_, in_=eidx.rearrange("p (a b) -> p a b", b=16)), ["posi"], ["St"])
                        for h in range(8):
                            for (sel, src_hc, dst) in ((af, 2 * h, ik), (bf_, 2 * h + 1, jk)):
                                E("vector", lambda e, sel=sel, h=h: e.tensor_tensor(
                                    out=oh, in0=sel[:, h, :].unsqueeze(2).broadcast_to([P, 16, 16]),
                                    in1=iota16[:].unsqueeze(1).broadcast_to([P, 16, 16]), op=ALU.is_equal), ["St", "iota16"], ["tq1"])
                                E("vector", lambda e, src_hc=src_hc: e.tensor_tensor(
                                    out=oh, in0=oh, in1=itf[:, src_hc, :].unsqueeze(1).broadcast_to([P, 16, 16]), op=ALU.mult),
                                    ["tq1", "St"], ["tq1"])
                                E("vector", lambda e, dst=dst, h=h: e.tensor_reduce(out=dst[:, h, :], in_=oh, axis=mybir.AxisListType.X,
                                                                                    op=ALU.add), ["tq1"], ["ang"])
                        E("vector", lambda e: e.scalar_tensor_tensor(out=ef, in0=ang[:, 0:128], scalar=128.0, in1=ang[:, 128:256],
                                                                     op0=ALU.mult, op1=ALU.add), ["ang"], ["ang"])
                        E("vector", lambda e: e.tensor_copy(out=eidx, in_=ef), ["ang"], ["posi"])
                        if debug and m == 0 and tt == 0:
                            D("sync", dbg["d_eidx"], eidx.bitcast(I32), reads=["posi"], is_output=True)
                            D("sync", dbg["d_gate"], Ct[:, 384:512], reads=["Ct"], is_output=True)
                        def gather(table, s_):
                            pg.dmaf("gpsimd", lambda e, table=table, s_=s_: e.indirect_dma_start(
                                out=gbuf, out_offset=None, in_=table,
                                in_offset=bass.IndirectOffsetOnAxis(ap=eidx[:, s_:s_ + 1], axis=0)),
                                reads=["posi"], writes=["uT"])
                        for s_ in range(128):
                            gather(expert_down, s_)
                            E("vector", lambda e, s_=s_: e.scalar_tensor_tensor(
                                out=junk[:], in0=gbuf, scalar=1.0, in1=h2tm[:], op0=ALU.mult, op1=ALU.mult,
                                accum_out=acol[:, s_:s_ + 1]), ["uT", "xnb1"], ["xnb0", "ang"])
                        ta = tm[:, 0:128]; tb = tm[:, 128:256]
                        E("vector", lambda e: e.tensor_tensor(out=ta, in0=acol, in1=acol, op=ALU.mult), ["ang"], ["tm"])
                        E("vector", lambda e: e.tensor_scalar(out=ta, in0=ta, scalar1=0.044715, scalar2=1.0,
                                                              op0=ALU.mult, op1=ALU.add), ["tm"], ["tm"])
                        E("vector", lambda e: e.tensor_tensor(out=ta, in0=ta, in1=acol, op=ALU.mult), ["tm", "ang"], ["tm"])
                        E("scalar", lambda e: e.activation(out=tb, in_=ta, func=AF.Tanh, scale=math.sqrt(2.0 / math.pi)),
                          ["tm"], ["tm"])
                        E("vector", lambda e: e.scalar_tensor_tensor(out=tb, in0=tb, scalar=1.0, in1=acol, op0=ALU.add,
                                                                     op1=ALU.mult), ["tm", "ang"], ["tm"])
                        E("vector", lambda e: e.scalar_tensor_tensor(out=wcol, in0=tb, scalar=0.5, in1=Ct[:, 384:512],
                                                                     op0=ALU.mult, op1=ALU.mult), ["tm", "Ct"], ["tq1"])
                        E("vector", lambda e: e.memset(acc_lo, 0.0), [], ["s_Xb"])
                        E("vector", lambda e: e.memset(acc_hi, 0.0), [], ["kT"])
                        for s_ in range(128):
                            gather(expert_up, s_)
                            E("vector", lambda e, s_=s_: e.scalar_tensor_tensor(
                                out=acc_lo, in0=gbuf[:, 0:1024], scalar=wcol[:, s_:s_ + 1], in1=acc_lo,
                                op0=ALU.mult, op1=ALU.add), ["uT", "tq1", "s_Xb"], ["s_Xb"])
                            E("vector", lambda e, s_=s_: e.scalar_tensor_tensor(
                                out=acc_hi, in0=gbuf[:, 1024:2048], scalar=wcol[:, s_:s_ + 1], in1=acc_hi,
                                op0=ALU.mult, op1=ALU.add), ["uT", "tq1", "kT"], ["kT"])
                        for j in range(16):
                            E("vector", lambda e, j=j: e.tensor_scalar(out=mtmp[:], in0=ident_f[:], scalar1=modT[:, 80 + j:81 + j],
                                                                       scalar2=None, op0=ALU.mult), ["ident_f", "modT"], ["mtmp"])
                            E("tensor", lambda e, j=j: e.matmul(psb[5][:, (j % 4) * P:(j % 4 + 1) * P], lhsT=ones_f[:], rhs=mtmp[:],
                                                                start=True, stop=True), ["ones_f", "mtmp"], ["psb5"])
                            if j % 4 == 3:
                                E("scalar", lambda e, j=j: e.activation(out=gbuf[:, (j - 3) * P:(j + 1) * P], in_=psb[5][:, :],
                                                                        func=AF.Identity), ["psb5"], ["uT"])
                        for (accp, akey, cs) in ((acc_lo, "s_Xb", slice(0, 1024)), (acc_hi, "kT", slice(1024, 2048))):
                            E("vector", lambda e, accp=accp, cs=cs: e.tensor_tensor(out=accp, in0=accp, in1=gbuf[:, cs], op=ALU.mult),
                              [akey, "uT"], [akey])
                            E("vector", lambda e, accp=accp, cs=cs: e.tensor_tensor(out=xb[0][:, cs], in0=xb[0][:, cs], in1=accp,
                                                                                    op=ALU.add), ["xb0", akey], ["xb0"])
                        if debug and m == 0 and tt == 0:
                            D("sync", dbg["d_x2"], xb[0][:], reads=["xb0"], is_output=True)
                    if not debug:
                        if tt == 0 or PEER_ON:
                            D("sync", gfin_u, g_final.partition_broadcast(P), writes=["uT"])
                        rms_stats(0, "xb0")
                        E("vector", lambda e: e.scalar_tensor_tensor(
                            out=xb[0][:], in0=xb[0][:], scalar=rstd[0][:], in1=gfin_u, op0=ALU.mult, op1=ALU.mult),
                            ["xb0", "rstd0", "uT"], ["xb0"])
                        D("sync", y[t * P:(t + 1) * P, :], xb[0][:], reads=["xb0"], is_output=True)

        gfin_b = wa[0][:, 0:8, :].rearrange("p a b -> p (a b)")
        if debug:
            D("sync", gfin_b, g_final.partition_broadcast(P), writes=["wa0"])
        for t in range(NT if debug else 0):
            b = t % 2
            D("sync", xb[b][:], x[t * P:(t + 1) * P, :], writes=["xb%d" % b])
            rms_stats(b, "xb%d" % b)
            E("vector", lambda e, b=b: e.scalar_tensor_tensor(
                out=xb[b][:], in0=xb[b][:], scalar=rstd[b][:], in1=gfin_b, op0=ALU.mult, op1=ALU.mult),
                ["xb%d" % b, "rstd%d" % b, "wa0"], ["xb%d" % b])
            D("sync", y[t * P:(t + 1) * P, :], xb[b][:], reads=["xb%d" % b], is_output=True)
        pg.finish()
        nc._pg_stats = pg.stats
    return nc


def col_layout(v, ncol):
    return np.ascontiguousarray(np.asarray(v, np.float32).reshape(ncol, P).T)


def make_in_maps(inputs, cores):
    consts = host_consts()
    L = 0
    b_in = np.asarray(inputs["b_in"][L], np.float32)
    bcol = np.zeros((P, 56), np.float32)
    bcol[:, 0:52] = col_layout(b_in, 52)
    for g in range(4):
        hb = b_in[1024 + g * 64:1024 + (g + 1) * 64]
        bcol[:, 52 + g] = np.concatenate([hb, hb])
    f32c = lambda a: np.ascontiguousarray(a, dtype=np.float32)
    def gp_l(a):
        return f32c(np.asarray(a).reshape(32, 2, 64).transpose(1, 2, 0).reshape(P, 32))
    ssm_l = {
        "ssm_are_l": gp_l(inputs["ssm_A_re"][L]), "ssm_aim_l": gp_l(inputs["ssm_A_im"][L]),
        "ssm_ldt_l": gp_l(np.repeat(np.asarray(inputs["ssm_log_dt"][L])[:, None], 64, axis=1)),
        "ssm_bre_l": f32c(np.asarray(inputs["ssm_B_re"][L]).reshape(32, 2, 64, 16).transpose(1, 2, 0, 3).reshape(P, 32, 16)),
        "ssm_bim_l": f32c(np.asarray(inputs["ssm_B_im"][L]).reshape(32, 2, 64, 16).transpose(1, 2, 0, 3).reshape(P, 32, 16)),
        "ssm_cre_l": f32c(np.asarray(inputs["ssm_C_re"][L]).reshape(32, 2, 16, 64).transpose(1, 3, 0, 2).reshape(P, 32, 16)),
        "ssm_cim_l": f32c(np.asarray(inputs["ssm_C_im"][L]).reshape(32, 2, 16, 64).transpose(1, 3, 0, 2).reshape(P, 32, 16)),
        "ssm_d_col": col_layout(inputs["ssm_D"][L], 8), "b_glu_col": col_layout(inputs["b_glu"][L], 8),
        "w_glu": f32c(inputs["w_glu"][L]),
        "w_attn_branch": f32c(inputs["w_attn_branch"][L]), "w_ssm_branch": f32c(inputs["w_ssm_branch"][L]),
        "w_out": f32c(inputs["w_out"][L]),
        "g_ffn_col": col_layout(inputs["g_ffn"][L], KC),
        "w_query": f32c(inputs["w_query"][L]),
        "sub_keys_l": f32c(np.asarray(inputs["sub_keys"][L]).reshape(16, P, P).transpose(2, 0, 1)),
        "expert_down": f32c(inputs["expert_down"][L]), "expert_up": f32c(inputs["expert_up"][L]),
    }
    maps = []
    for c in cores:
        m = {
            "x": np.ascontiguousarray(inputs["x"][c], dtype=np.float32),
            "pos": np.ascontiguousarray(inputs["positions"][c], dtype=np.int32).reshape(1, SEQ),
            "c_col": col_layout(inputs["c"][c], KC),
            "w_ada": np.ascontiguousarray(inputs["w_ada"][L], dtype=np.float32),
            "b_ada_col": col_layout(inputs["b_ada"][L], 96),
            "g_mix_col": col_layout(inputs["g_mix"][L], KC),
            "w_in": np.ascontiguousarray(inputs["w_in"][L], dtype=np.float32),
            "b_in_col": bcol,
            "g_final": np.ascontiguousarray(inputs["g_final"], dtype=np.float32).reshape(1, D_MODEL),
            "attn_sinks": np.ascontiguousarray(inputs["attn_sinks"][L], dtype=np.float32).reshape(1, 16),
        }
        m.update(ssm_l)
        m.update(consts)
        maps.append(m)
    return maps


_NC_CACHE = {}


def kernel(**inputs):
    if "nc" not in _NC_CACHE:
        _NC_CACHE["nc"] = build_nc()
    nc = _NC_CACHE["nc"]
    cores = list(range(N_CORES))
    in_maps = make_in_maps(inputs, cores)
    res = run_bass_kernel_spmd(nc, in_maps, core_ids=cores)
    return np.stack([np.asarray(r["y"]) for r in res.results], axis=0).astype(np.float32)
```

```python
import math
import os
from contextlib import ExitStack

import numpy as np
import concourse.bass as bass
import concourse.mybir as mybir
from concourse.bass_utils import run_bass_kernel_spmd

F32 = mybir.dt.float32
BF16 = mybir.dt.bfloat16
I32 = mybir.dt.int32
U32 = mybir.dt.uint32
AF = mybir.ActivationFunctionType
ALU = mybir.AluOpType

STAGES_DONE = ["adaLN-mod", "norm1+modulate", "in-proj(q,k,v,u,gates)", "rope", "swa-attention+sinks", "s5-ssm+glu",
               "gated-merge", "w_out+gate1+residual", "final-rmsnorm"]
STAGES_MISSING = []

D_MODEL = 2048
SEQ = 2048
N_CORES = 8
RMS_EPS = 1e-6
P = 128
NT = SEQ // P
KC = D_MODEL // P
MT = 512
NM = SEQ // MT
IN_COLS = 6656
ROPE_THETA = 500000.0
TWO_PI = 2.0 * math.pi

ENGS = ("sync", "tensor", "vector", "scalar", "gpsimd")
N_DMA_SEMS = 12


class Prog:
    def __init__(self, nc, stack):
        self.nc = nc
        self.stack = stack
        self.ins = []
        self.per_eng = {e: [] for e in ENGS}
        self.res = {}
        self.out_dmas = []

    def _deps(self, reads, writes):
        deps = set()
        for k in reads:
            r = self.res.get(k)
            if r and r[0] is not None:
                deps.add(r[0])
        for k in writes:
            r = self.res.get(k)
            if r:
                if r[0] is not None:
                    deps.add(r[0])
                deps.update(r[1])
        return deps

    def _commit(self, iid, reads, writes):
        for k in reads:
            self.res.setdefault(k, [None, set()])[1].add(iid)
        for k in writes:
            self.res[k] = [iid, set()]

    def emit(self, eng, fn, reads=(), writes=()):
        deps = self._deps(reads, writes)
        iid = len(self.ins)
        if eng == "tensor":
            deps = {d for d in deps if self.ins[d]["eng"] != "tensor" or self.ins[d]["dma"]}
        self.ins.append(dict(eng=eng, fn=fn, deps=deps, dma=False, waited=False))
        self.per_eng[eng].append(iid)
        self._commit(iid, reads, writes)
        return iid

    def dma(self, eng, out, in_, reads=(), writes=(), is_output=False, **kw):
        deps = self._deps(reads, writes)
        iid = len(self.ins)
        fn = lambda e, out=out, in_=in_, kw=kw: e.dma_start(out=out, in_=in_, **kw)
        self.ins.append(dict(eng=eng, fn=fn, deps=deps, dma=True, waited=False))
        self.per_eng[eng].append(iid)
        self._commit(iid, reads, writes)
        if is_output:
            self.out_dmas.append(iid)
        return iid

    def dmaf(self, eng, fn, reads=(), writes=(), is_output=False, slot=0):
        deps = self._deps(reads, writes)
        iid = len(self.ins)
        self.ins.append(dict(eng=eng, fn=fn, deps=deps, dma=True, waited=False, sw=True, slot=slot))
        self.per_eng[eng].append(iid)
        self._commit(iid, reads, writes)
        if is_output:
            self.out_dmas.append(iid)
        return iid

    def finish(self):
        nc = self.nc
        ins = self.ins
        fin = len(ins)
        ins.append(dict(eng="sync", fn=None, deps=set(self.out_dmas), dma=False, waited=False))
        self.per_eng["sync"].append(fin)
        for r in ins:
            for d in r["deps"]:
                ins[d]["waited"] = True
        EPOCH = 2000
        DEPOCH = 120
        sems = {}

        def sem_of(key):
            if key not in sems:
                sems[key] = self.stack.enter_context(nc.semaphore("m_" + "_".join(str(k) for k in key)))
            return sems[key]

        ecnt = {e: 0 for e in ENGS}
        ngather = {}
        duse = [0] * N_DMA_SEMS
        dlast = [None] * N_DMA_SEMS
        nd = 0
        for iid, r in enumerate(ins):
            if r["dma"] and r.get("sw"):
                sl = r["slot"]
                ep, u = divmod(ngather.setdefault(sl, 0), 200)
                ngather[sl] += 1
                r["ev"] = (("g", sl, ep), 16 * (u + 1))
            elif r["dma"]:
                slot = nd % N_DMA_SEMS
                nd += 1
                if dlast[slot] is not None:
                    r["deps"].add(dlast[slot])
                    ins[dlast[slot]]["waited"] = True
                dlast[slot] = iid
                ep, u = divmod(duse[slot], DEPOCH)
                duse[slot] += 1
                r["ev"] = (("d", slot, ep), 16 * (u + 1))
            elif r["waited"]:
                ep, u = divmod(ecnt[r["eng"]], EPOCH)
                ecnt[r["eng"]] += 1
                r["ev"] = (("e", r["eng"], ep), u + 1)
            else:
                r["ev"] = None
        self.stats = dict(n_ins=len(ins), ecnt=dict(ecnt), duse=list(duse))

        with nc.Block() as block:
            def make(engname):
                def body(eng):
                    seen = {}
                    for iid in self.per_eng[engname]:
                        r = ins[iid]
                        need = {}
                        for d in r["deps"]:
                            key, val = ins[d]["ev"]
                            if seen.get(key, 0) >= val:
                                continue
                            need[key] = max(need.get(key, 0), val)
                        for key, val in need.items():
                            eng.wait_ge(sem_of(key), val)
                            seen[key] = val
                        if r["fn"] is None:
                            continue
                        bi = r["fn"](eng)
                        if r["ev"] is not None:
                            key, val = r["ev"]
                            bi.then_inc(sem_of(key), 16 if key[0] in ("d", "g") else 1)
                return body
            block.sync(make("sync"))
            block.tensor(make("tensor"))
            block.vector(make("vector"))
            block.scalar(make("scalar"))
            block.gpsimd(make("gpsimd"))


def host_consts():
    c = {}
    c["ident_f"] = np.eye(P, dtype=np.float32)
    pm = np.zeros((P, P), np.float32)
    for h0 in (0, 64):
        for d in range(8):
            pm[h0 + d + 8, h0 + d] = -1.0
            pm[h0 + d, h0 + d + 8] = 1.0
    c["rope_pm"] = pm
    invf = np.zeros((P, 1), np.float32)
    f = ROPE_THETA ** (-(np.arange(0, 16, 2, dtype=np.float32) / 16.0))
    for h0 in (0, 64):
        for d in range(16):
            invf[h0 + d, 0] = f[d % 8]
    c["rope_invf"] = invf.astype(np.float32)
    tk = np.arange(P)[:, None]; tq = np.arange(P)[None, :]
    rm = np.zeros((P, 2), np.float32); rm[:64, 0] = 1.0; rm[64:, 1] = 1.0
    c["rowmask"] = rm
    c["ones_f"] = np.ones((P, P), np.float32)
    c["iota16"] = np.tile(np.arange(16, dtype=np.float32)[None, :], (P, 1))
    c["mask_cur"] = (tk <= tq).astype(np.float32)
    c["mask_prev"] = (tk > tq).astype(np.float32)
    return c


def build_nc(debug=False, upto=9):
    nc = bass.Bass("TRN2", target_bir_lowering=False)

    def din(name, shape, dt=F32):
        return nc.dram_tensor(name, list(shape), dt, kind="ExternalInput").ap()

    x = din("x", [SEQ, D_MODEL])
    pos = din("pos", [1, SEQ], I32)
    c_col = din("c_col", [P, KC])
    w_ada = din("w_ada", [D_MODEL, 6 * D_MODEL])
    b_ada_col = din("b_ada_col", [P, 96])
    g_mix_col = din("g_mix_col", [P, KC])
    w_in = din("w_in", [D_MODEL, IN_COLS])
    b_in_col = din("b_in_col", [P, 56])
    g_final = din("g_final", [1, D_MODEL])
    ident_f_d = din("ident_f", [P, P])
    rope_pm_d = din("rope_pm", [P, P])
    rope_invf_d = din("rope_invf", [P, 1])
    mask_cur_d = din("mask_cur", [P, P])
    mask_prev_d = din("mask_prev", [P, P])
    sinks_d = din("attn_sinks", [1, 16])
    rowmask_d = din("rowmask", [P, 2])
    ssm_are_d = din("ssm_are_l", [P, 32]); ssm_aim_d = din("ssm_aim_l", [P, 32]); ssm_ldt_d = din("ssm_ldt_l", [P, 32])
    ssm_bre_d = din("ssm_bre_l", [P, 32, 16]); ssm_bim_d = din("ssm_bim_l", [P, 32, 16])
    ssm_cre_d = din("ssm_cre_l", [P, 32, 16]); ssm_cim_d = din("ssm_cim_l", [P, 32, 16])
    ssm_d_col_d = din("ssm_d_col", [P, 8]); b_glu_col_d = din("b_glu_col", [P, 8])
    w_glu = din("w_glu", [1024, 1024])
    w_ab = din("w_attn_branch", [1024, D_MODEL]); w_sb = din("w_ssm_branch", [1024, D_MODEL])
    w_out = din("w_out", [D_MODEL, D_MODEL])
    ones_d = din("ones_f", [P, P])
    g_ffn_col_d = din("g_ffn_col", [P, KC])
    w_query = din("w_query", [D_MODEL, D_MODEL])
    sk_l_d = din("sub_keys_l", [P, 16, P])
    iota16_d = din("iota16", [P, 16])
    expert_down = din("expert_down", [16384, D_MODEL])
    expert_up = din("expert_up", [16384, D_MODEL])
    y = nc.dram_tensor("y", [SEQ, D_MODEL], F32, kind="ExternalOutput").ap()
    dbg = {}
    if debug:
        for nm, shp in (("d_mod", [P, 96]), ("d_hT", [P, 8, MT]), ("d_q", [P, 8, MT]),
                        ("d_k", [P, 4, MT]), ("d_v", [P, 2, MT]), ("d_u", [P, 8, MT]), ("d_att", [P, 8, MT]), ("d_abar", [P, 64]), ("d_y", [P, 8, MT]), ("d_ssm", [P, 8, MT]), ("d_mrg", [P, 8, MT]), ("d_x1", [P, D_MODEL]), ("d_eidx", [P, P]), ("d_gate", [P, P]), ("d_x2", [P, D_MODEL])):
            dbg[nm] = nc.dram_tensor(nm, shp, (I32 if nm == "d_eidx" else F32) if nm in ("d_mod", "d_abar", "d_x1", "d_eidx", "d_gate", "d_x2") else BF16, kind="ExternalOutput").ap()

    w_ada_v = w_ada.rearrange("(kc p) n -> p kc n", p=P)
    w_in_v = w_in.rearrange("(kc p) n -> p kc n", p=P)

    with ExitStack() as st:
        def sb(name, shape, dt=F32):
            return st.enter_context(nc.sbuf_tensor("sb_" + name, list(shape), dt))

        def ps(name, shape, dt=F32):
            return st.enter_context(nc.psum_tensor(name, list(shape), dt))

        pg = Prog(nc, st)
        E, D = pg.emit, pg.dma

        ident_f = sb("ident_f", [P, P]); D("sync", ident_f[:], ident_f_d, writes=["ident_f"])
        ident_b = sb("ident_b", [P, P], BF16)
        E("vector", lambda e: e.tensor_copy(out=ident_b[:], in_=ident_f[:]), ["ident_f"], ["ident_b"])
        pm_f = sb("pm_f", [P, P]); D("sync", pm_f[:], rope_pm_d, writes=["pm_f"])
        pm_b = sb("pm_b", [P, P], BF16)
        E("vector", lambda e: e.tensor_copy(out=pm_b[:], in_=pm_f[:]), ["pm_f"], ["pm_b"])
        invf = sb("invf", [P, 1]); D("sync", invf[:], rope_invf_d, writes=["invf"])
        ccol = sb("ccol", [P, KC]); D("sync", ccol[:], c_col, writes=["ccol"])
        bada = sb("bada", [P, 96]); D("sync", bada[:], b_ada_col, writes=["bada"])
        gmix = sb("gmix", [P, KC]); D("sync", gmix[:], g_mix_col, writes=["gmix"])
        binc = sb("binc", [P, 56]); D("sync", binc[:], b_in_col, writes=["binc"])

        mtmp = sb("mtmp", [P, P])
        mcur = sb("mcur", [P, P], BF16); mprev = sb("mprev", [P, P], BF16)
        D("sync", mtmp[:], mask_cur_d, writes=["mtmp"])
        E("vector", lambda e: e.tensor_copy(out=mcur[:], in_=mtmp[:]), ["mtmp"], ["mcur"])
        D("sync", mtmp[:], mask_prev_d, writes=["mtmp"])
        E("vector", lambda e: e.tensor_copy(out=mprev[:], in_=mtmp[:]), ["mtmp"], ["mprev"])
        rowmask = sb("rowmask", [P, 2]); D("sync", rowmask[:], rowmask_d, writes=["rowmask"])
        ones_f = sb("ones_f", [P, P]); D("sync", ones_f[:], ones_d, writes=["ones_f"])
        esink = sb("esink", [P, 16])
        D("sync", esink[:], sinks_d.partition_broadcast(P), writes=["esink"])
        E("scalar", lambda e: e.activation(out=esink[:], in_=esink[:], func=AF.Exp), ["esink"], ["esink"])
        psb = [ps("psb%d" % i, [P, 512]) for i in range(6)]
        psT = ps("psT", [P, 2048], BF16)
        psk = ["psb%d" % i for i in range(6)]

        silc = sb("silc", [P, KC])
        E("scalar", lambda e: e.activation(out=silc[:], in_=ccol[:], func=AF.Silu), ["ccol"], ["silc"])
        wa = [sb("wa%d" % i, [P, KC, 256]) for i in range(2)]
        modT = sb("modT", [P, 96])
        for blk in range(48):
            b = blk % 2
            D("sync", wa[b][:], w_ada_v[:, :, blk * 256:(blk + 1) * 256], writes=["wa%d" % b])
            for sub in range(2):
                j = blk * 2 + sub
                for kc in range(KC):
                    E("tensor", lambda e, b=b, sub=sub, j=j, kc=kc: e.matmul(
                        psb[0][:, j:j + 1], lhsT=wa[b][:, kc, sub * 128:(sub + 1) * 128],
                        rhs=silc[:, kc:kc + 1], start=(kc == 0), stop=(kc == KC - 1)),
                        ["wa%d" % b, "silc"], ["psb0"])
        E("vector", lambda e: e.tensor_tensor(out=modT[:], in0=psb[0][:, 0:96], in1=bada[:], op=ALU.add),
          ["psb0", "bada"], ["modT"])
        A1 = sb("A1", [P, KC])
        E("vector", lambda e: e.scalar_tensor_tensor(out=A1[:], in0=modT[:, 16:32], scalar=1.0, in1=gmix[:],
                                                     op0=ALU.add, op1=ALU.mult), ["modT", "gmix"], ["A1"])
        if debug:
            D("sync", dbg["d_mod"], modT[:], reads=["modT"], is_output=True)

        xb = [sb("xb%d" % i, [P, D_MODEL]) for i in range(2)]
        ss = [sb("ss%d" % i, [P, 1]) for i in range(2)]
        rstd = [sb("rstd%d" % i, [P, 1]) for i in range(2)]
        xnb = [sb("xnb%d" % i, [P, D_MODEL], BF16) for i in range(2)]
        hT = sb("hT", [P, KC, MT], BF16)
        wr = [sb("wr%d" % i, [P, KC, 256], BF16) for i in range(2)]
        qT = sb("qT", [P, 8, MT], BF16)
        kT = sb("kT", [P, 4, MT], BF16)
        vT = sb("vT", [P, 2, MT], BF16)
        uT = sb("uT", [P, 8, MT], BF16)
        raw = [sb("raw%d" % i, [P, MT], BF16) for i in range(2)]
        posi = sb("posi", [P, MT], I32)
        ang = sb("ang", [P, MT]); tq1 = sb("tq1", [P, MT]); tqi = posi
        tm = sb("tm", [P, MT])
        Ct = sb("Ct", [P, MT]); St = sb("St", [P, MT])
        t1 = tq1; t2 = tm
        kprev = sb("kprev", [P, 2, 4, P], BF16)
        kz = sb("kz", [P, 2, 4, MT], BF16)
        E("vector", lambda e: e.memset(kprev[:], 0.0), [], ["kprev"])
        v_tm = sb("v_tm", [P, 5, 4, 65], BF16)
        E("vector", lambda e: e.memset(v_tm[:], 1.0), [], ["v_tm"])
        pebuf = [sb("pe%d" % i, [P, 4 * P], BF16) for i in range(4)]
        den = sb("den", [P, 4])
        attn_tm = sb("attn_tm", [P, 1024], BF16)
        aoT = sb("aoT", [P, 8, MT], BF16)
        acnt = [0]
        wcount = [0]

        def rms_stats(b, src_key):
            E("scalar", lambda e, b=b: e.activation(out=xnb[b][:], in_=xb[b][:], func=AF.Square, accum_out=ss[b][:]),
              [src_key], ["xnb%d" % b, "ss%d" % b])
            E("scalar", lambda e, b=b: e.activation(out=rstd[b][:], in_=ss[b][:], func=AF.Sqrt,
                                                    scale=1.0 / D_MODEL, bias=RMS_EPS),
              ["ss%d" % b], ["rstd%d" % b])
            E("vector", lambda e, b=b: e.reciprocal(out=rstd[b][:], in_=rstd[b][:]),
              ["rstd%d" % b], ["rstd%d" % b])

        def reduce_angle(src_ap_fn, dst, shift):
            E("vector", lambda e: e.tensor_scalar(out=tq1[:], in0=ang[:], scalar1=shift, scalar2=1.0 / TWO_PI,
                                                  op0=ALU.add, op1=ALU.mult), ["ang"], ["tq1"])
            E("vector", lambda e: e.tensor_copy(out=tqi[:], in_=tq1[:]), ["tq1"], ["posi"])
            E("vector", lambda e: e.tensor_copy(out=tq1[:], in_=tqi[:]), ["posi"], ["tq1"])
            E("vector", lambda e: e.tensor_scalar(out=tm[:], in0=ang[:], scalar1=shift, scalar2=None,
                                                  op0=ALU.add), ["ang"], ["tm"])
            E("vector", lambda e: e.scalar_tensor_tensor(out=tm[:], in0=tq1[:], scalar=-TWO_PI, in1=tm[:],
                                                         op0=ALU.mult, op1=ALU.add), ["tq1", "tm"], ["tm"])
            E("vector", lambda e: e.tensor_scalar(out=tq1[:], in0=tm[:], scalar1=math.pi, scalar2=-TWO_PI,
                                                  op0=ALU.is_gt, op1=ALU.mult), ["tm"], ["tq1"])
            E("vector", lambda e: e.tensor_tensor(out=tm[:], in0=tm[:], in1=tq1[:], op=ALU.add), ["tm", "tq1"], ["tm"])
            E("vector", lambda e: e.tensor_scalar(out=tq1[:], in0=tm[:], scalar1=-math.pi, scalar2=TWO_PI,
                                                  op0=ALU.is_lt, op1=ALU.mult), ["tm"], ["tq1"])
            E("vector", lambda e: e.tensor_tensor(out=tm[:], in0=tm[:], in1=tq1[:], op=ALU.add), ["tm", "tq1"], ["tm"])
            E("scalar", lambda e: e.activation(out=dst[:], in_=tm[:], func=AF.Sin), ["tm"], [dst_key(dst)])

        keymap = {}

        def dst_key(t):
            return keymap[id(t)]
        keymap[id(Ct)] = "Ct"; keymap[id(St)] = "St"

        def load_w(cols):
            b = wcount[0] % 2
            wcount[0] += 1
            for (d0, s0, n) in cols:
                D("sync", wa[b][:, :, d0:d0 + n], w_in_v[:, :, s0:s0 + n], writes=["wa%d" % b])
            E("gpsimd", lambda e, b=b: e.tensor_copy(out=wr[b][:], in_=wa[b][:]), ["wa%d" % b], ["wr%d" % b])
            return b

        pcount = [0]

        def proj_chunk(wb, wc0, bias_col, dst_ap, dst_key_, rope):
            bank = 1 + (pcount[0] % 3)
            pcount[0] += 1
            for kc in range(KC):
                E("tensor", lambda e, kc=kc, bank=bank: e.matmul(
                    psb[bank][:, :], lhsT=wr[wb][:, kc, wc0:wc0 + 128], rhs=hT[:, kc, :],
                    start=(kc == 0), stop=(kc == KC - 1)), ["wr%d" % wb, "hT"], [psk[bank]])
            if not rope:
                E("scalar", lambda e, bank=bank: e.activation(out=dst_ap, in_=psb[bank][:, :], func=AF.Identity,
                                                              bias=binc[:, bias_col:bias_col + 1], scale=1.0),
                  [psk[bank], "binc"], [dst_key_])
                return
            rb = pcount[0] % 2
            E("scalar", lambda e, bank=bank, rb=rb: e.activation(out=raw[rb][:], in_=psb[bank][:, :], func=AF.Identity,
                                                                 bias=binc[:, bias_col:bias_col + 1], scale=1.0),
              [psk[bank], "binc"], ["raw%d" % rb])
            pb = 4 + (pcount[0] % 2)
            E("tensor", lambda e, pb=pb, rb=rb: e.matmul(psb[pb][:, :], lhsT=pm_b[:], rhs=raw[rb][:],
                                                         start=True, stop=True), ["pm_b", "raw%d" % rb], [psk[pb]])
            E("vector", lambda e, rb=rb: e.tensor_tensor(out=t1[:], in0=raw[rb][:], in1=Ct[:], op=ALU.mult),
              ["raw%d" % rb, "Ct"], ["tq1"])
            E("vector", lambda e, pb=pb: e.tensor_tensor(out=t2[:], in0=psb[pb][:, :], in1=St[:], op=ALU.mult),
              [psk[pb], "St"], ["tm"])
            E("vector", lambda e: e.tensor_tensor(out=dst_ap, in0=t1[:], in1=t2[:], op=ALU.add),
              ["tq1", "tm"], [dst_key_])


        SSM_ON = upto >= 5
        SSM_RUN = upto >= 6
        if SSM_ON:
            w_glu_v = w_glu.rearrange("(kc p) n -> p kc n", p=P)
            are = sb("s_are", [P, 32]); aim = sb("s_aim", [P, 32]); ldt = sb("s_ldt", [P, 32])
            D("sync", are[:], ssm_are_d, writes=["s_are"]); D("sync", aim[:], ssm_aim_d, writes=["s_aim"])
            D("sync", ldt[:], ssm_ldt_d, writes=["s_ldt"])
            dcol = sb("s_dcol", [P, 8]); D("sync", dcol[:], ssm_d_col_d, writes=["s_dcol"])
            bglu = sb("s_bglu", [P, 8]); D("sync", bglu[:], b_glu_col_d, writes=["s_bglu"])
            sv = {}; svk = {}
            def S(name):
                if name not in sv:
                    i = len(sv)
                    base = Ct if i < 16 else St
                    sv[name] = base[:, (i % 16) * 32:(i % 16 + 1) * 32]
                    svk[name] = "Ct" if i < 16 else "St"
                return sv[name]
            def SK(name):
                S(name)
                return svk[name]
            def V(op, o, a, b_=None, **kw):
                if b_ is None:
                    E("vector", lambda e, o=o, a=a, kw=kw: e.tensor_scalar(out=S(o), in0=S(a), **kw), [SK(a)], [SK(o)])
                else:
                    E("vector", lambda e, o=o, a=a, b_=b_, op=op: e.tensor_tensor(out=S(o), in0=S(a), in1=S(b_), op=op),
                      [SK(a), SK(b_)], [SK(o)])
            sv["are"], sv["aim"], sv["ldt"] = are[:], aim[:], ldt[:]
            svk["are"], svk["aim"], svk["ldt"] = "s_are", "s_aim", "s_ldt"
            E("scalar", lambda e: e.activation(out=S("dt"), in_=ldt[:], func=AF.Exp), ["s_ldt"], [SK("dt")])
            V(None, "lre", "are", scalar1=-1e-4, scalar2=None, op0=ALU.min)
            V(ALU.mult, "lrdt", "lre", "dt")
            E("scalar", lambda e: e.activation(out=S("mag"), in_=S("lrdt"), func=AF.Exp), [SK("lrdt")], [SK("mag")])
            V(ALU.mult, "th", "aim", "dt")
            sv["qi"] = posi[:, 0:32]; svk["qi"] = "posi"
            def sincos(dst, shift):
                V(None, "q1", "th", scalar1=shift, scalar2=1.0 / TWO_PI, op0=ALU.add, op1=ALU.mult)
                qi = sv["qi"]
                E("vector", lambda e: e.tensor_copy(out=qi, in_=S("q1")), [SK("q1")], ["posi"])
                E("vector", lambda e: e.tensor_copy(out=S("q1"), in_=qi), ["posi"], [SK("q1")])
                V(None, "r", "th", scalar1=shift, scalar2=None, op0=ALU.add)
                E("vector", lambda e: e.scalar_tensor_tensor(out=S("r"), in0=S("q1"), scalar=-TWO_PI, in1=S("r"),
                                                             op0=ALU.mult, op1=ALU.add), [SK("q1"), SK("r")], [SK("r")])
                V(None, "q1", "r", scalar1=math.pi, scalar2=-TWO_PI, op0=ALU.is_gt, op1=ALU.mult)
                V(ALU.add, "r", "r", "q1")
                V(None, "q1", "r", scalar1=-math.pi, scalar2=TWO_PI, op0=ALU.is_lt, op1=ALU.mult)
                V(ALU.add, "r", "r", "q1")
                E("scalar", lambda e: e.activation(out=S(dst)[:], in_=S("r"), func=AF.Sin), [SK("r")], [SK(dst)])
            sincos("sn", 0.0)
            sincos("cs", math.pi / 2.0)
            V(ALU.mult, "abr", "mag", "cs")
            V(ALU.mult, "abi", "mag", "sn")
            V(ALU.mult, "t_a", "lre", "lre"); V(ALU.mult, "t_b", "aim", "aim"); V(ALU.add, "den", "t_a", "t_b")
            E("vector", lambda e: e.reciprocal(out=S("rden"), in_=S("den")), [SK("den")], [SK("rden")])
            V(None, "nre", "abr", scalar1=-1.0, scalar2=None, op0=ALU.add)
            V(ALU.mult, "t_a", "nre", "lre"); V(ALU.mult, "t_b", "abi", "aim"); V(ALU.add, "zre", "t_a", "t_b")
            V(ALU.mult, "zre", "zre", "rden")
            V(ALU.mult, "t_a", "abi", "lre"); V(ALU.mult, "t_b", "nre", "aim"); V(ALU.subtract, "zim", "t_a", "t_b")
            V(ALU.mult, "zim", "zim", "rden")
            V(None, "nzim", "zim", scalar1=-1.0, scalar2=None, op0=ALU.mult)
            V(None, "nabi", "abi", scalar1=-1.0, scalar2=None, op0=ALU.mult)
            if debug:
                dab = sb("s_dab", [P, 64])
                E("vector", lambda e: e.tensor_copy(out=dab[:, 0:32], in_=S("abr")), [SK("abr")], ["s_dab"])
                E("vector", lambda e: e.tensor_copy(out=dab[:, 32:64], in_=S("abi")), [SK("abi")], ["s_dab"])
                D("sync", dbg["d_abar"], dab[:], reads=["s_dab"], is_output=True)
            hTf = hT[:].bitcast(F32)
            def scr(i):
                return hTf[:, 2 * i:2 * i + 2, :].rearrange("p a (b c) -> p (a b) c", c=16)
            bre, bim = scr(0), scr(1)
            D("sync", bre[:], ssm_bre_d, writes=["hT"]); D("sync", bim[:], ssm_bim_d, writes=["hT"])
            bbr, bbi, btmp = scr(2), scr(3), scr(4)
            def bc(n):
                return S(n).unsqueeze(2).broadcast_to([P, 32, 16])
            E("vector", lambda e: e.tensor_tensor(out=bbr[:], in0=bre[:], in1=bc("zre"), op=ALU.mult), ["hT", SK("zre")], ["hT"])
            E("vector", lambda e: e.tensor_tensor(out=btmp[:], in0=bim[:], in1=bc("nzim"), op=ALU.mult), ["hT", SK("nzim")], ["hT"])
            E("vector", lambda e: e.tensor_tensor(out=bbr[:], in0=bbr[:], in1=btmp[:], op=ALU.add), ["hT", "hT"], ["hT"])
            E("vector", lambda e: e.tensor_tensor(out=bbi[:], in0=bim[:], in1=bc("zre"), op=ALU.mult), ["hT", SK("zre")], ["hT"])
            E("vector", lambda e: e.tensor_tensor(out=btmp[:], in0=bre[:], in1=bc("zim"), op=ALU.mult), ["hT", SK("zim")], ["hT"])
            E("vector", lambda e: e.tensor_tensor(out=bbi[:], in0=bbi[:], in1=btmp[:], op=ALU.add), ["hT", "hT"], ["hT"])
            LB = sb("s_LB", [P, 64, P], BF16)
            bsrc = sb("s_bsrc", [P, P])
            for k in range(32):
                for ri, bb in ((0, bbr), (1, bbi)):
                    E("vector", lambda e: e.memset(bsrc[:], 0.0), [], ["s_bsrc"])
                    for g2 in range(2):
                        c0 = (k % 4) * 32 + g2 * 16
                        E("vector", lambda e, bb=bb, k=k, g2=g2, c0=c0: e.tensor_scalar(
                            out=bsrc[:, c0:c0 + 16], in0=bb[:, k, :], scalar1=rowmask[:, g2:g2 + 1], scalar2=None,
                            op0=ALU.mult), ["hT", "hT", "rowmask"], ["s_bsrc"])
                    E("tensor", lambda e: e.transpose(out=psb[0][:, 0:P], in_=bsrc[:], identity=ident_f[:]),
                      ["s_bsrc", "ident_f"], ["psb0"])
                    E("scalar", lambda e, k=k, ri=ri: e.activation(out=LB[:, 2 * k + ri, :], in_=psb[0][:, 0:P],
                                                                   func=AF.Identity), ["psb0"], ["s_LB"])
            cre, cim = scr(5), scr(6)
            D("sync", cre[:], ssm_cre_d, writes=["hT"]); D("sync", cim[:], ssm_cim_d, writes=["hT"])
            CP = sb("s_CP", [P, 64, P], BF16)
            E("vector", lambda e: e.memset(CP[:], 0.0), [], ["s_CP"])
            for k in range(32):
                for ri, cc, sgn in ((0, cre, 1.0), (1, cim, -1.0)):
                    for g2 in range(2):
                        c0 = (k % 4) * 32 + g2 * 16
                        E("vector", lambda e, cc=cc, k=k, ri=ri, g2=g2, c0=c0, sgn=sgn: e.tensor_scalar(
                            out=CP[:, 2 * k + ri, c0:c0 + 16], in0=cc[:, k, :], scalar1=rowmask[:, g2:g2 + 1],
                            scalar2=sgn, op0=ALU.mult, op1=ALU.mult), ["hT", "hT", "rowmask"], ["s_CP"])
            Aar = sb("s_Aar", [P, 2, 32]); Aai = sb("s_Aai", [P, 2, 32])
            for ri in range(2):
                E("vector", lambda e, ri=ri: e.tensor_copy(out=Aar[:, ri, :], in_=S("abr")), [SK("abr")], ["s_Aar"])
            E("vector", lambda e: e.tensor_copy(out=Aai[:, 0, :], in_=S("nabi")), [SK("nabi")], ["s_Aai"])
            E("vector", lambda e: e.tensor_copy(out=Aai[:, 1, :], in_=S("abi")), [SK("abi")], ["s_Aai"])
            LCH = 32
            BUs = xb[0][:].rearrange("p (r k t) -> p r k t", r=2, k=32)
            Xh = xb[1][:].rearrange("p (r k t) -> p r k t", r=2, k=32)
            Xb = sb("s_Xb", [P, 2, 32, LCH], BF16)
            Xst = sb("s_Xst", [P, 2, 32])
            E("vector", lambda e: e.memset(Xst[:], 0.0), [], ["s_Xst"])
            T1 = sb("s_T1", [P, 2, 32]); T2 = sb("s_T2", [P, 2, 32])
            yT = qT
            zT = kz[:].rearrange("p a g t -> p (a g) t")
            soT = sb("s_soT", [P, 8, MT], BF16)

        PEER_ON = upto >= 8
        if PEER_ON:
            w_query_v = w_query.rearrange("(kc p) n -> p kc n", p=P)
            gffn = sb("gffn", [P, KC]); D("sync", gffn[:], g_ffn_col_d, writes=["gffn"])
            A2 = sb("A2", [P, KC])
            E("vector", lambda e: e.scalar_tensor_tensor(out=A2[:], in0=modT[:, 64:80], scalar=1.0, in1=gffn[:],
                                                         op0=ALU.add, op1=ALU.mult), ["modT", "gffn"], ["A2"])
            iota16 = sb("iota16", [P, 16]); D("sync", iota16[:], iota16_d, writes=["iota16"])
            skT = sb("skT", [P, 16, P], BF16)
            D("sync", wa[0][:, :, 0:P], sk_l_d, writes=["wa0"])
            E("vector", lambda e: e.tensor_copy(out=skT[:], in_=wa[0][:, :, 0:P]), ["wa0"], ["skT"])
        n_macro = (1 if upto >= 1 else 0) if debug else NM
        for m in range(n_macro):
            for tl in range(4):
                t = m * 4 + tl
                b = t % 2
                D("sync", xb[b][:], x[t * P:(t + 1) * P, :], writes=["xb%d" % b])
                rms_stats(b, "xb%d" % b)
                E("vector", lambda e, b=b: e.tensor_scalar(out=xnb[b][:], in0=xb[b][:], scalar1=rstd[b][:],
                                                           scalar2=None, op0=ALU.mult),
                  ["xb%d" % b, "rstd%d" % b], ["xnb%d" % b])
                for kc in range(KC):
                    E("tensor", lambda e, b=b, kc=kc: e.transpose(
                        out=psT[:, kc * P:(kc + 1) * P], in_=xnb[b][:, kc * P:(kc + 1) * P], identity=ident_b[:]),
                        ["xnb%d" % b, "ident_b"], ["psT"])
                for kc in range(KC):
                    E("vector", lambda e, kc=kc, tl=tl: e.tensor_scalar(
                        out=hT[:, kc, tl * P:(tl + 1) * P], in0=psT[:, kc * P:(kc + 1) * P],
                        scalar1=A1[:, kc:kc + 1], scalar2=modT[:, kc:kc + 1], op0=ALU.mult, op1=ALU.add),
                        ["psT", "A1", "modT"], ["hT"])
            if upto < 2:
                if debug:
                    D('sync', dbg['d_hT'], hT[:, 0:8, :], reads=['hT'], is_output=True)
                continue
            D("sync", posi[:], pos[:, m * MT:(m + 1) * MT].partition_broadcast(P), writes=["posi"])
            E("vector", lambda e: e.tensor_copy(out=ang[:], in_=posi[:]), ["posi"], ["ang"])
            E("vector", lambda e: e.tensor_scalar(out=ang[:], in0=ang[:], scalar1=invf[:, 0:1], scalar2=None,
                                                  op0=ALU.mult), ["ang", "invf"], ["ang"])
            reduce_angle(None, St, 0.0)
            reduce_angle(None, Ct, math.pi / 2.0)
            if upto < 3:
                continue
            for blk in range(4):
                wb = load_w([(0, blk * 256, 256)])
                for sub in range(2):
                    j = blk * 2 + sub
                    proj_chunk(wb, sub * 128, j, qT[:, j, :], "qT", True)
            for gp in range(2):
                g0, g1 = 2 * gp, 2 * gp + 1
                wb = load_w([(0, 1024 + g0 * 64, 64), (64, 1024 + g0 * 64, 64),
                             (128, 1024 + g1 * 64, 64), (192, 1024 + g1 * 64, 64)])
                proj_chunk(wb, 0, 52 + g0, kT[:, g0, :], "kT", True)
                proj_chunk(wb, 128, 52 + g1, kT[:, g1, :], "kT", True)
            wb = load_w([(0, 1280, 256)])
            for sub in range(2):
                proj_chunk(wb, sub * 128, 10 + sub, vT[:, sub, :], "vT", False)
            for blk in range(4):
                wb = load_w([(0, 1536 + blk * 256, 256)])
                for sub in range(2):
                    j = blk * 2 + sub
                    proj_chunk(wb, sub * 128, 12 + j, uT[:, j, :], "uT", False)
            if upto >= 4:
                for hl in range(2):
                    E("vector", lambda e, hl=hl: e.tensor_scalar(
                        out=kz[:, hl, :, :], in0=kT[:, :, :], scalar1=rowmask[:, hl:hl + 1], scalar2=None,
                        op0=ALU.mult), ["kT", "rowmask"], ["kz"])
                for blk in range(4):
                    for c2 in range(2):
                        E("tensor", lambda e, blk=blk, c2=c2: e.transpose(
                            out=psT[:, c2 * P:(c2 + 1) * P], in_=vT[:, c2, blk * P:(blk + 1) * P], identity=ident_b[:]),
                            ["vT", "ident_b"], ["psT"])
                    E("vector", lambda e, blk=blk: e.tensor_copy(
                        out=v_tm[:, 1 + blk, :, 0:64], in_=psT[:, 0:256].rearrange("p (g d) -> p g d", d=64)),
                        ["psT"], ["v_tm"])
                ATT_SUB = int(os.environ.get("ATT_SUB", "9"))
                for b in range(4 if ATT_SUB >= 2 else 0):
                    gb = 4 * m + b
                    for g in range(4):
                        kbs = (["prev"] if gb > 0 else []) + ["cur"]
                        pes = []
                        for kb in kbs:
                            bank = 1 + (acnt[0] % 3)
                            pi = acnt[0] % 4
                            acnt[0] += 1
                            for i in range(4):
                                hq = 4 * g + i
                                chunk = hq // 2
                                base = (hq % 2) * 64
                                hl = hq % 2
                                if kb == "cur":
                                    ksrc = kz[:, hl, g, b * P:(b + 1) * P]; kkey = "kz"
                                elif b > 0:
                                    ksrc = kz[:, hl, g, (b - 1) * P:b * P]; kkey = "kz"
                                else:
                                    ksrc = kprev[:, hl, g, :]; kkey = "kprev"
                                E("tensor", lambda e, bank=bank, i=i, ksrc=ksrc, chunk=chunk, b=b: e.matmul(
                                    psb[bank][:, i * P:(i + 1) * P], lhsT=ksrc,
                                    rhs=qT[:, chunk, b * P:(b + 1) * P], start=True, stop=True),
                                    [kkey, "qT"], [psk[bank]])
                            pe = pebuf[pi]
                            E("scalar", lambda e, pe=pe, bank=bank: e.activation(
                                out=pe[:], in_=psb[bank][:, :], func=AF.Exp, scale=0.125), [psk[bank]], ["pe%d" % pi])
                            mk = mcur if kb == "cur" else mprev
                            mkey = "mcur" if kb == "cur" else "mprev"
                            E("vector", lambda e, pe=pe, mk=mk: e.tensor_tensor(
                                out=pe[:].rearrange("p (h q) -> p h q", q=P), in0=pe[:].rearrange("p (h q) -> p h q", q=P),
                                in1=mk[:].unsqueeze(1).broadcast_to([P, 4, P]), op=ALU.mult),
                                ["pe%d" % pi, mkey], ["pe%d" % pi])
                            slot = (1 + b) if kb == "cur" else b
                            pes.append((pe, "pe%d" % pi, slot))
                        pob = 4 + (acnt[0] % 2)
                        if ATT_SUB < 3:
                            continue
                        for i in range(4):
                            for n, (pe, pkey, slot) in enumerate(pes):
                                E("tensor", lambda e, pob=pob, i=i, pe=pe, slot=slot, g=g, n=n, L=len(pes): e.matmul(
                                    psb[pob][:, i * 65:(i + 1) * 65], lhsT=pe[:, i * P:(i + 1) * P],
                                    rhs=v_tm[:, slot, g, :], start=(n == 0), stop=(n == L - 1)),
                                    [pkey, "v_tm"], [psk[pob]])
                        if ATT_SUB < 4:
                            continue
                        po3 = psb[pob][:, 0:260].rearrange("p (h d) -> p h d", d=65)
                        E("vector", lambda e, po3=po3, g=g: e.tensor_tensor(
                            out=den[:], in0=po3[:, :, 64], in1=esink[:, 4 * g:4 * g + 4], op=ALU.add),
                            [psk[pob], "esink"], ["den"])
                        E("vector", lambda e: e.reciprocal(out=den[:], in_=den[:]), ["den"], ["den"])
                        E("vector", lambda e, po3=po3, g=g: e.tensor_tensor(
                            out=attn_tm[:, 4 * g * 64:(4 * g + 4) * 64].rearrange("p (h d) -> p h d", d=64),
                            in0=po3[:, :, 0:64], in1=den[:].unsqueeze(2).broadcast_to([P, 4, 64]), op=ALU.mult),
                            [psk[pob], "den"], ["attn_tm"])
                    for c in range(8 if ATT_SUB >= 5 else 0):
                        E("tensor", lambda e, c=c: e.transpose(
                            out=psT[:, c * P:(c + 1) * P], in_=attn_tm[:, c * P:(c + 1) * P], identity=ident_b[:]),
                            ["attn_tm", "ident_b"], ["psT"])
                    E("vector", lambda e, b=b: e.tensor_copy(
                        out=aoT[:, :, b * P:(b + 1) * P], in_=psT[:, 0:1024].rearrange("p (c t) -> p c t", t=P)),
                        ["psT"], ["aoT"])
                for hl in range(2):
                    E("vector", lambda e, hl=hl: e.tensor_copy(out=kprev[:, hl, :, :], in_=kz[:, hl, :, 3 * P:4 * P]),
                      ["kz"], ["kprev"])
                E("vector", lambda e: e.tensor_copy(out=v_tm[:, 0, :, :], in_=v_tm[:, 4, :, :]), ["v_tm"], ["v_tm"])
                if debug and m == 0:
                    D("sync", dbg["d_att"], aoT[:], reads=["aoT"], is_output=True)
            if debug and m == 0:
                for nm, src, key, n in (("d_q", qT, "qT", 8), ("d_k", kT, "kT", 4),
                                        ("d_v", vT, "vT", 2), ("d_u", uT, "uT", 8)):
                    D("sync", dbg[nm], src[:], reads=[key], is_output=True)

            if SSM_ON and SSM_RUN:
                NCH = MT // LCH
                for c in range(NCH):
                    tc0 = c * LCH
                    for ri in range(2):
                        for half in range(2):
                            bank = 1 + 2 * ri + half
                            for kk in range(16):
                                k = half * 16 + kk
                                E("tensor", lambda e, bank=bank, kk=kk, k=k, ri=ri, tc0=tc0: e.matmul(
                                    psb[bank][:, kk * LCH:(kk + 1) * LCH], lhsT=LB[:, 2 * k + ri, :],
                                    rhs=uT[:, k // 4, tc0:tc0 + LCH], start=True, stop=True),
                                    ["s_LB", "uT"], [psk[bank]])
                            E("scalar", lambda e, bank=bank, ri=ri, half=half: e.activation(
                                out=BUs[:, ri, half * 16:(half + 1) * 16, :],
                                in_=psb[bank][:, :].rearrange("p (k t) -> p k t", t=LCH), func=AF.Identity),
                                [psk[bank]], ["xb0"])
                    for sidx in range(LCH):
                        if c == 0 and sidx == 0:
                            prev = Xst; pkey = "s_Xst"
                            pv = lambda r_: Xst[:, r_, :]
                            pall = Xst[:]
                        else:
                            col = (sidx - 1) % LCH
                            pkey = "xb1"
                            pv = lambda r_, col=col: Xh[:, r_, :, col]
                            pall = Xh[:, :, :, col]
                        E("vector", lambda e, pall=pall: e.tensor_tensor(out=T1[:], in0=Aar[:], in1=pall, op=ALU.mult),
                          ["s_Aar", pkey], ["s_T1"])
                        E("vector", lambda e, pv=pv: e.tensor_tensor(out=T2[:, 0, :], in0=Aai[:, 0, :], in1=pv(1), op=ALU.mult),
                          ["s_Aai", pkey], ["s_T2"])
                        E("vector", lambda e, pv=pv: e.tensor_tensor(out=T2[:, 1, :], in0=Aai[:, 1, :], in1=pv(0), op=ALU.mult),
                          ["s_Aai", pkey], ["s_T2"])
                        E("vector", lambda e: e.tensor_tensor(out=T1[:], in0=T1[:], in1=T2[:], op=ALU.add),
                          ["s_T1", "s_T2"], ["s_T1"])
                        E("vector", lambda e, sidx=sidx: e.tensor_tensor(out=Xh[:, :, :, sidx], in0=T1[:], in1=BUs[:, :, :, sidx],
                                                                         op=ALU.add), ["s_T1", "xb0"], ["xb1"])
                    for ri in range(2):
                        E("gpsimd", lambda e, ri=ri: e.tensor_copy(out=Xb[:, ri, :, :], in_=Xh[:, ri, :, :]), ["xb1"], ["s_Xb"])
                    ybank = 5
                    for kt in range(8):
                        n = 0
                        for kk in range(4):
                            k = 4 * kt + kk
                            for ri in range(2):
                                E("tensor", lambda e, kt=kt, k=k, ri=ri, n=n: e.matmul(
                                    psb[ybank][:, kt * LCH:(kt + 1) * LCH], lhsT=CP[:, 2 * k + ri, :],
                                    rhs=Xb[:, ri, k, :], start=(n == 0), stop=(n == 7)), ["s_CP", "s_Xb"], [psk[ybank]])
                                n += 1
                    for kt in range(8):
                        E("vector", lambda e, kt=kt, tc0=tc0: e.scalar_tensor_tensor(
                            out=yT[:, kt, tc0:tc0 + LCH], in0=uT[:, kt, tc0:tc0 + LCH], scalar=dcol[:, kt:kt + 1],
                            in1=psb[ybank][:, kt * LCH:(kt + 1) * LCH], op0=ALU.mult, op1=ALU.add),
                            ["uT", "s_dcol", psk[ybank]], ["qT"])
                E("vector", lambda e: e.tensor_copy(out=Xst[:], in_=Xh[:, :, :, LCH - 1]), ["xb1"], ["s_Xst"])
                if debug and m == 0:
                    D("sync", dbg["d_y"], yT[:], reads=["qT"], is_output=True)
                for kt in range(8):
                    xk = yT[:, kt, :]
                    E("vector", lambda e, xk=xk: e.tensor_tensor(out=tq1[:], in0=xk, in1=xk, op=ALU.mult), ["qT"], ["tq1"])
                    E("vector", lambda e: e.tensor_scalar(out=tq1[:], in0=tq1[:], scalar1=0.044715, scalar2=1.0,
                                                          op0=ALU.mult, op1=ALU.add), ["tq1"], ["tq1"])
                    E("vector", lambda e, xk=xk: e.tensor_tensor(out=tq1[:], in0=tq1[:], in1=xk, op=ALU.mult), ["tq1", "qT"], ["tq1"])
                    E("scalar", lambda e: e.activation(out=tm[:], in_=tq1[:], func=AF.Tanh, scale=math.sqrt(2.0 / math.pi)),
                      ["tq1"], ["tm"])
                    E("vector", lambda e, xk=xk: e.scalar_tensor_tensor(out=tm[:], in0=tm[:], scalar=1.0, in1=xk,
                                                                        op0=ALU.add, op1=ALU.mult), ["tm", "qT"], ["tm"])
                    E("scalar", lambda e, kt=kt: e.activation(out=zT[:, kt, :], in_=tm[:], func=AF.Identity, scale=0.5),
                      ["tm"], ["kz"])
                for blk in range(4):
                    b = wcount[0] % 2
                    wcount[0] += 1
                    D("sync", wa[b][:, 0:8, :], w_glu_v[:, :, blk * 256:(blk + 1) * 256], writes=["wa%d" % b])
                    E("gpsimd", lambda e, b=b: e.tensor_copy(out=wr[b][:, 0:8, :], in_=wa[b][:, 0:8, :]), ["wa%d" % b], ["wr%d" % b])
                    for sub in range(2):
                        j = blk * 2 + sub
                        bank = 1 + (pcount[0] % 3)
                        pcount[0] += 1
                        for kc in range(8):
                            E("tensor", lambda e, kc=kc, bank=bank, b=b, sub=sub: e.matmul(
                                psb[bank][:, :], lhsT=wr[b][:, kc, sub * P:(sub + 1) * P], rhs=zT[:, kc, :],
                                start=(kc == 0), stop=(kc == 7)), ["wr%d" % b, "kz"], [psk[bank]])
                        E("scalar", lambda e, bank=bank, j=j: e.activation(out=tq1[:], in_=psb[bank][:, :], func=AF.Sigmoid,
                                                                           bias=bglu[:, j:j + 1], scale=1.0),
                          [psk[bank], "s_bglu"], ["tq1"])
                        E("vector", lambda e, j=j: e.tensor_tensor(out=soT[:, j, :], in0=zT[:, j, :], in1=tq1[:], op=ALU.mult),
                          ["kz", "tq1"], ["s_soT"])
                if debug and m == 0:
                    D("sync", dbg["d_ssm"], soT[:], reads=["s_soT"], is_output=True)

            if upto >= 7:
                w_ab_v = w_ab.rearrange("(kc p) n -> p kc n", p=P)
                w_sb_v = w_sb.rearrange("(kc p) n -> p kc n", p=P)
                w_out_v = w_out.rearrange("(kc p) n -> p kc n", p=P)
                mg = [qT, kz[:].rearrange("p a g t -> p (a g) t")]
                mgk = ["qT", "kz"]
                for j in range(16):
                    ba = wcount[0] % 2; wcount[0] += 1
                    D("sync", wa[ba][:, 0:8, 0:P], w_ab_v[:, :, j * P:(j + 1) * P], writes=["wa%d" % ba])
                    D("sync", wa[ba][:, 0:8, P:2 * P], w_sb_v[:, :, j * P:(j + 1) * P], writes=["wa%d" % ba])
                    E("gpsimd", lambda e, ba=ba: e.tensor_copy(out=wr[ba][:, 0:8, :], in_=wa[ba][:, 0:8, :]),
                      ["wa%d" % ba], ["wr%d" % ba])
                    for kc in range(8):
                        E("tensor", lambda e, kc=kc, ba=ba: e.matmul(psb[1][:, :], lhsT=wr[ba][:, kc, 0:P], rhs=aoT[:, kc, :],
                                                                     start=(kc == 0), stop=(kc == 7)), ["wr%d" % ba, "aoT"], ["psb1"])
                    for kc in range(8):
                        E("tensor", lambda e, kc=kc, ba=ba: e.matmul(psb[2][:, :], lhsT=wr[ba][:, kc, P:2 * P], rhs=soT[:, kc, :],
                                                                     start=(kc == 0), stop=(kc == 7)), ["wr%d" % ba, "s_soT"], ["psb2"])
                    bg = wcount[0] % 2; wcount[0] += 1
                    D("sync", wa[bg][:, :, 0:P], w_in_v[:, :, 2560 + j * P:2560 + (j + 1) * P], writes=["wa%d" % bg])
                    D("sync", wa[bg][:, :, P:2 * P], w_in_v[:, :, 4608 + j * P:4608 + (j + 1) * P], writes=["wa%d" % bg])
                    E("gpsimd", lambda e, bg=bg: e.tensor_copy(out=wr[bg][:], in_=wa[bg][:]), ["wa%d" % bg], ["wr%d" % bg])
                    for kc in range(KC):
                        E("tensor", lambda e, kc=kc, bg=bg: e.matmul(psb[3][:, :], lhsT=wr[bg][:, kc, 0:P], rhs=hT[:, kc, :],
                                                                     start=(kc == 0), stop=(kc == KC - 1)), ["wr%d" % bg, "hT"], ["psb3"])
                    for kc in range(KC):
                        E("tensor", lambda e, kc=kc, bg=bg: e.matmul(psb[4][:, :], lhsT=wr[bg][:, kc, P:2 * P], rhs=hT[:, kc, :],
                                                                     start=(kc == 0), stop=(kc == KC - 1)), ["wr%d" % bg, "hT"], ["psb4"])
                    E("scalar", lambda e, j=j: e.activation(out=tq1[:], in_=psb[3][:, :], func=AF.Sigmoid,
                                                            bias=binc[:, 20 + j:21 + j], scale=1.0), ["psb3", "binc"], ["tq1"])
                    E("scalar", lambda e, j=j: e.activation(out=tm[:], in_=psb[4][:, :], func=AF.Sigmoid,
                                                            bias=binc[:, 36 + j:37 + j], scale=1.0), ["psb4", "binc"], ["tm"])
                    E("vector", lambda e: e.tensor_tensor(out=tq1[:], in0=tq1[:], in1=psb[1][:, :], op=ALU.mult), ["tq1", "psb1"], ["tq1"])
                    E("vector", lambda e: e.tensor_tensor(out=tm[:], in0=tm[:], in1=psb[2][:, :], op=ALU.mult), ["tm", "psb2"], ["tm"])
                    E("vector", lambda e, j=j: e.tensor_tensor(out=mg[j // 8][:, j % 8, :], in0=tq1[:], in1=tm[:], op=ALU.add),
                      ["tq1", "tm"], [mgk[j // 8]])
                if debug and m == 0:
                    D("sync", dbg["d_mrg"], mg[0][:], reads=["qT"], is_output=True)
                g1b = xb[1]
                gfin_u = uT[:].bitcast(F32).rearrange("p a b -> p (a b)")
                for j in range(16):
                    E("vector", lambda e, j=j: e.tensor_scalar(out=mtmp[:], in0=ident_f[:], scalar1=modT[:, 32 + j:33 + j],
                                                               scalar2=None, op0=ALU.mult), ["ident_f", "modT"], ["mtmp"])
                    E("tensor", lambda e, j=j: e.matmul(psb[5][:, (j % 4) * P:(j % 4 + 1) * P], lhsT=ones_f[:], rhs=mtmp[:],
                                                        start=True, stop=True), ["ones_f", "mtmp"], ["psb5"])
                    if j % 4 == 3:
                        E("scalar", lambda e, j=j: e.activation(out=g1b[:, (j - 3) * P:(j + 1) * P], in_=psb[5][:, :],
                                                                func=AF.Identity), ["psb5"], ["xb1"])
                for tt in range(4):
                    t = m * 4 + tt
                    D("sync", xb[0][:], x[t * P:(t + 1) * P, :], writes=["xb0"])
                    for blk in range(8):
                        bo = wcount[0] % 2; wcount[0] += 1
                        D("sync", wa[bo][:], w_out_v[:, :, blk * 256:(blk + 1) * 256], writes=["wa%d" % bo])
                        E("gpsimd", lambda e, bo=bo: e.tensor_copy(out=wr[bo][:], in_=wa[bo][:]), ["wa%d" % bo], ["wr%d" % bo])
                        bank = 1 + (pcount[0] % 3); pcount[0] += 1
                        for kc in range(KC):
                            E("tensor", lambda e, kc=kc, bo=bo, bank=bank, tt=tt: e.matmul(
                                psb[bank][:, 0:256], lhsT=mg[kc // 8][:, kc % 8, tt * P:(tt + 1) * P], rhs=wr[bo][:, kc, :],
                                start=(kc == 0), stop=(kc == KC - 1)), ["wr%d" % bo, "qT", "kz"], [psk[bank]])
                        cs = slice(blk * 256, (blk + 1) * 256)
                        E("vector", lambda e, bank=bank, cs=cs: e.tensor_tensor(out=tq1[:, 0:256], in0=psb[bank][:, 0:256],
                                                                               in1=g1b[:, cs], op=ALU.mult), [psk[bank], "xb1"], ["tq1"])
                        E("vector", lambda e, cs=cs: e.tensor_tensor(out=xb[0][:, cs], in0=xb[0][:, cs], in1=tq1[:, 0:256],
                                                                     op=ALU.add), ["xb0", "tq1"], ["xb0"])
                    if debug and m == 0 and tt == 0:
                        D("sync", dbg["d_x1"], xb[0][:], reads=["xb0"], is_output=True)
                    if PEER_ON:
                        h2T = hT[:, :, 0:P]; qpT = hT[:, :, P:2 * P]; h2tm = xnb[1]; junk = xnb[0]
                        sc = aoT[:].bitcast(F32).rearrange("p a (b c) -> p (a b) c", c=P)
                        sc2 = soT[:].bitcast(F32).rearrange("p a (b c) -> p (a b) c", c=P)
                        cand = soT[:].bitcast(F32)
                        vt = Ct[:, 0:256].rearrange("p (a b) -> p a b", b=16)
                        ctv = Ct[:, 256:384].rearrange("p (a b) -> p a b", b=16)
                        gt = Ct[:, 384:512].rearrange("p (a b) -> p a b", b=16)
                        itf = St[:, 0:256].rearrange("p (a b) -> p a b", b=16)
                        af = St[:, 256:384].rearrange("p (a b) -> p a b", b=16)
                        bf_ = St[:, 384:512].rearrange("p (a b) -> p a b", b=16)
                        ik = ang[:, 0:128].rearrange("p (a b) -> p a b", b=16)
                        jk = ang[:, 128:256].rearrange("p (a b) -> p a b", b=16)
                        ef = ang[:, 256:384]; acol = ang[:, 384:512]
                        pu = posi[:].bitcast(U32)
                        it = pu[:, 0:256].rearrange("p (a b) -> p a b", b=16)
                        ci = pu[:, 256:384].rearrange("p (a b) -> p a b", b=16)
                        eidx = pu[:, 384:512]
                        oh = tq1[:, 0:256].rearrange("p (a b) -> p a b", b=16)
                        wcol = tq1[:, 256:384]; gsm = tq1[:, 384:392]
                        gbuf = uT[:].bitcast(F32).rearrange("p a b -> p (a b)")
                        acc_lo = Xb[:].bitcast(F32).rearrange("p r k t -> p (r k t)")
                        acc_hi = kT[:].bitcast(F32).rearrange("p g t -> p (g t)")
                        rms_stats(0, "xb0")
                        E("vector", lambda e: e.tensor_scalar(out=xnb[0][:], in0=xb[0][:], scalar1=rstd[0][:], scalar2=None,
                                                              op0=ALU.mult), ["xb0", "rstd0"], ["xnb0"])
                        for kc in range(KC):
                            E("tensor", lambda e, kc=kc: e.transpose(out=psT[:, kc * P:(kc + 1) * P],
                                                                     in_=xnb[0][:, kc * P:(kc + 1) * P], identity=ident_b[:]),
                              ["xnb0", "ident_b"], ["psT"])
                        for kc in range(KC):
                            E("vector", lambda e, kc=kc: e.tensor_scalar(
                                out=h2T[:, kc, :], in0=psT[:, kc * P:(kc + 1) * P], scalar1=A2[:, kc:kc + 1],
                                scalar2=modT[:, 48 + kc:49 + kc], op0=ALU.mult, op1=ALU.add), ["psT", "A2", "modT"], ["hT"])
                        for kc in range(KC):
                            E("tensor", lambda e, kc=kc: e.transpose(out=psT[:, kc * P:(kc + 1) * P], in_=h2T[:, kc, :],
                                                                     identity=ident_b[:]), ["hT", "ident_b"], ["psT"])
                        E("vector", lambda e: e.tensor_copy(out=h2tm[:], in_=psT[:, :]), ["psT"], ["xnb1"])
                        for blk in range(8):
                            bq = wcount[0] % 2; wcount[0] += 1
                            D("sync", wa[bq][:], w_query_v[:, :, blk * 256:(blk + 1) * 256], writes=["wa%d" % bq])
                            E("gpsimd", lambda e, bq=bq: e.tensor_copy(out=wr[bq][:], in_=wa[bq][:]), ["wa%d" % bq], ["wr%d" % bq])
                            for sub in range(2):
                                hc = blk * 2 + sub
                                bank = 1 + (pcount[0] % 3); pcount[0] += 1
                                for kc in range(KC):
                                    E("tensor", lambda e, kc=kc, bq=bq, sub=sub, bank=bank: e.matmul(
                                        psb[bank][:, 0:P], lhsT=wr[bq][:, kc, sub * P:(sub + 1) * P], rhs=h2T[:, kc, :],
                                        start=(kc == 0), stop=(kc == KC - 1)), ["wr%d" % bq, "hT"], [psk[bank]])
                                E("scalar", lambda e, hc=hc, bank=bank: e.activation(out=qpT[:, hc, :], in_=psb[bank][:, 0:P],
                                                                                     func=AF.Identity), [psk[bank]], ["hT"])
                        for hc in range(16):
                            E("tensor", lambda e, hc=hc: e.matmul(psb[4][:, (hc % 4) * P:(hc % 4 + 1) * P], lhsT=qpT[:, hc, :],
                                                                  rhs=skT[:, hc, :], start=True, stop=True), ["hT", "skT"], ["psb4"])
                            if hc % 4 == 3:
                                E("scalar", lambda e, hc=hc: e.activation(
                                    out=sc[:, hc - 3:hc + 1, :], in_=psb[4][:, :].rearrange("p (a b) -> p a b", b=P),
                                    func=AF.Identity), ["psb4"], ["aoT"])
                        for hc in range(16):
                            E("vector", lambda e, hc=hc: e.max(out=vt[:, hc, 0:8], in_=sc[:, hc, :]), ["aoT"], ["Ct"])
                            E("vector", lambda e, hc=hc: e.max_index(out=it[:, hc, 0:8], in_max=vt[:, hc, 0:8], in_values=sc[:, hc, :]),
                              ["aoT", "Ct"], ["posi"])
                            E("vector", lambda e, hc=hc: e.match_replace(out=sc2[:, hc, :], in_to_replace=vt[:, hc, 0:8],
                                                                         in_values=sc[:, hc, :], imm_value=-1e30),
                              ["aoT", "Ct"], ["s_soT"])
                            E("vector", lambda e, hc=hc: e.max(out=vt[:, hc, 8:16], in_=sc2[:, hc, :]), ["s_soT"], ["Ct"])
                            E("vector", lambda e, hc=hc: e.max_index(out=it[:, hc, 8:16], in_max=vt[:, hc, 8:16], in_values=sc2[:, hc, :]),
                              ["s_soT", "Ct"], ["posi"])
                        E("vector", lambda e: e.tensor_copy(out=itf, in_=it), ["posi"], ["St"])
                        for h in range(8):
                            E("vector", lambda e, h=h: e.tensor_tensor(
                                out=cand[:, h, :].rearrange("p (a b) -> p a b", b=16),
                                in0=vt[:, 2 * h, :].unsqueeze(2).broadcast_to([P, 16, 16]),
                                in1=vt[:, 2 * h + 1, :].unsqueeze(1).broadcast_to([P, 16, 16]), op=ALU.add), ["Ct"], ["s_soT"])
                        for h in range(8):
                            E("vector", lambda e, h=h: e.max(out=ctv[:, h, 0:8], in_=cand[:, h, :]), ["s_soT"], ["Ct"])
                            E("vector", lambda e, h=h: e.max_index(out=ci[:, h, 0:8], in_max=ctv[:, h, 0:8], in_values=cand[:, h, :]),
                              ["s_soT", "Ct"], ["posi"])
                            E("vector", lambda e, h=h: e.match_replace(out=cand[:, h, :], in_to_replace=ctv[:, h, 0:8],
                                                                       in_values=cand[:, h, :], imm_value=-1e30),
                              ["s_soT", "Ct"], ["s_soT"])
                            E("vector", lambda e, h=h: e.max(out=ctv[:, h, 8:16], in_=cand[:, h, :]), ["s_soT"], ["Ct"])
                            E("vector", lambda e, h=h: e.max_index(out=ci[:, h, 8:16], in_max=ctv[:, h, 8:16], in_values=cand[:, h, :]),
                              ["s_soT", "Ct"], ["posi"])
                        E("vector", lambda e: e.tensor_tensor(out=gt, in0=ctv, in1=ctv[:, :, 0:1].broadcast_to([P, 8, 16]),
                                                              op=ALU.subtract), ["Ct"], ["Ct"])
                        E("scalar", lambda e: e.activation(out=gt, in_=gt, func=AF.Exp), ["Ct"], ["Ct"])
                        E("vector", lambda e: e.tensor_reduce(out=gsm, in_=gt, axis=mybir.AxisListType.X, op=ALU.add), ["Ct"], ["tq1"])
                        E("vector", lambda e: e.reciprocal(out=gsm, in_=gsm), ["tq1"], ["tq1"])
                        E("vector", lambda e: e.tensor_tensor(out=gt, in0=gt, in1=gsm.unsqueeze(2).broadcast_to([P, 8, 16]),
                                                              op=ALU.mult), ["Ct", "tq1"], ["Ct"])
                        E("vector", lambda e: e.tensor_single_scalar(out=eidx.rearrange("p (a b) -> p a b", b=16), in_=ci, scalar=4,
                                                                      op=ALU.logical_shift_right), ["posi"], ["posi"])
                        E("vector", lambda e: e.tensor_copy(out=af, in_=eidx.rearrange("p (a b) -> p a b", b=16)), ["posi"], ["St"])
                        E("vector", lambda e: e.tensor_single_scalar(out=eidx.rearrange("p (a b) -> p a b", b=16), in_=ci, scalar=15,
                                                                      op=ALU.bitwise_and), ["posi"], ["posi"])
                        E("vector", lambda e: e.tensor_copy(out=bf_, in_=eidx.rearrange("p (a b) -> p a b", b=16)), ["posi"], ["St"])
                        for h in range(8):
                            for (sel, src_hc, dst) in ((af, 2 * h, ik), (bf_, 2 * h + 1, jk)):
                                E("vector", lambda e, sel=sel, h=h: e.tensor_tensor(
                                    out=oh, in0=sel[:, h, :].unsqueeze(2).broadcast_to([P, 16, 16]),
                                    in1=iota16[:].unsqueeze(1).broadcast_to([P, 16, 16]), op=ALU.is_equal), ["St", "iota16"], ["tq1"])
                                E("vector", lambda e, src_hc=src_hc: e.tensor_tensor(
                                    out=oh, in0=oh, in1=itf[:, src_hc, :].unsqueeze(1).broadcast_to([P, 16, 16]), op=ALU.mult),
                                    ["tq1", "St"], ["tq1"])
                                E("vector", lambda e, dst=dst, h=h: e.tensor_reduce(out=dst[:, h, :], in_=oh, axis=mybir.AxisListType.X,
                                                                                    op=ALU.add), ["tq1"], ["ang"])
                        E("vector", lambda e: e.scalar_tensor_tensor(out=ef, in0=ang[:, 0:128], scalar=128.0, in1=ang[:, 128:256],
                                                                     op0=ALU.mult, op1=ALU.add), ["ang"], ["ang"])
                        E("vector", lambda e: e.tensor_copy(out=eidx, in_=ef), ["ang"], ["posi"])
                        if debug and m == 0 and tt == 0:
                            D("sync", dbg["d_eidx"], eidx.bitcast(I32), reads=["posi"], is_output=True)
                            D("sync", dbg["d_gate"], Ct[:, 384:512], reads=["Ct"], is_output=True)
                        gbufs = [gbuf, aoT[:].bitcast(F32).rearrange("p a b -> p (a b)"),
                                 soT[:].bitcast(F32).rearrange("p a b -> p (a b)")]
                        gkeys = ["uT", "aoT", "s_soT"]
                        LOOK = 2
                        def gather(table, s_, sl):
                            pg.dmaf("gpsimd", lambda e, table=table, s_=s_, sl=sl: e.indirect_dma_start(
                                out=gbufs[sl], out_offset=None, in_=table,
                                in_offset=bass.IndirectOffsetOnAxis(ap=eidx[:, s_:s_ + 1], axis=0)),
                                reads=["posi"], writes=[gkeys[sl]], slot=sl)
                        for s_ in range(min(LOOK, 128)):
                            gather(expert_down, s_, s_ % 3)
                        for s_ in range(128):
                            if s_ + LOOK < 128:
                                gather(expert_down, s_ + LOOK, (s_ + LOOK) % 3)
                            sl = s_ % 3
                            E("vector", lambda e, s_=s_, sl=sl: e.scalar_tensor_tensor(
                                out=junk[:], in0=gbufs[sl], scalar=1.0, in1=h2tm[:], op0=ALU.mult, op1=ALU.mult,
                                accum_out=acol[:, s_:s_ + 1]), [gkeys[sl], "xnb1"], ["xnb0", "ang"])
                        ta = tm[:, 0:128]; tb = tm[:, 128:256]
                        E("vector", lambda e: e.tensor_tensor(out=ta, in0=acol, in1=acol, op=ALU.mult), ["ang"], ["tm"])
                        E("vector", lambda e: e.tensor_scalar(out=ta, in0=ta, scalar1=0.044715, scalar2=1.0,
                                                              op0=ALU.mult, op1=ALU.add), ["tm"], ["tm"])
                        E("vector", lambda e: e.tensor_tensor(out=ta, in0=ta, in1=acol, op=ALU.mult), ["tm", "ang"], ["tm"])
                        E("scalar", lambda e: e.activation(out=tb, in_=ta, func=AF.Tanh, scale=math.sqrt(2.0 / math.pi)),
                          ["tm"], ["tm"])
                        E("vector", lambda e: e.scalar_tensor_tensor(out=tb, in0=tb, scalar=1.0, in1=acol, op0=ALU.add,
                                                                     op1=ALU.mult), ["tm", "ang"], ["tm"])
                        E("vector", lambda e: e.scalar_tensor_tensor(out=wcol, in0=tb, scalar=0.5, in1=Ct[:, 384:512],
                                                                     op0=ALU.mult, op1=ALU.mult), ["tm", "Ct"], ["tq1"])
                        E("vector", lambda e: e.memset(acc_lo, 0.0), [], ["s_Xb"])
                        E("vector", lambda e: e.memset(acc_hi, 0.0), [], ["kT"])
                        for s_ in range(min(LOOK, 128)):
                            gather(expert_up, s_, (128 + s_) % 3)
                        for s_ in range(128):
                            if s_ + LOOK < 128:
                                gather(expert_up, s_ + LOOK, (128 + s_ + LOOK) % 3)
                            sl = (128 + s_) % 3
                            E("vector", lambda e, s_=s_, sl=sl: e.scalar_tensor_tensor(
                                out=acc_lo, in0=gbufs[sl][:, 0:1024], scalar=wcol[:, s_:s_ + 1], in1=acc_lo,
                                op0=ALU.mult, op1=ALU.add), [gkeys[sl], "tq1", "s_Xb"], ["s_Xb"])
                            E("vector", lambda e, s_=s_, sl=sl: e.scalar_tensor_tensor(
                                out=acc_hi, in0=gbufs[sl][:, 1024:2048], scalar=wcol[:, s_:s_ + 1], in1=acc_hi,
                                op0=ALU.mult, op1=ALU.add), [gkeys[sl], "tq1", "kT"], ["kT"])
                        for j in range(16):
                            E("vector", lambda e, j=j: e.tensor_scalar(out=mtmp[:], in0=ident_f[:], scalar1=modT[:, 80 + j:81 + j],
                                                                       scalar2=None, op0=ALU.mult), ["ident_f", "modT"], ["mtmp"])
                            E("tensor", lambda e, j=j: e.matmul(psb[5][:, (j % 4) * P:(j % 4 + 1) * P], lhsT=ones_f[:], rhs=mtmp[:],
                                                                start=True, stop=True), ["ones_f", "mtmp"], ["psb5"])
                            if j % 4 == 3:
                                E("scalar", lambda e, j=j: e.activation(out=gbuf[:, (j - 3) * P:(j + 1) * P], in_=psb[5][:, :],
                                                                        func=AF.Identity), ["psb5"], ["uT"])
                        for (accp, akey, cs) in ((acc_lo, "s_Xb", slice(0, 1024)), (acc_hi, "kT", slice(1024, 2048))):
                            E("vector", lambda e, accp=accp, cs=cs: e.tensor_tensor(out=accp, in0=accp, in1=gbuf[:, cs], op=ALU.mult),
                              [akey, "uT"], [akey])
                            E("vector", lambda e, accp=accp, cs=cs: e.tensor_tensor(out=xb[0][:, cs], in0=xb[0][:, cs], in1=accp,
                                                                                    op=ALU.add), ["xb0", akey], ["xb0"])
                        if debug and m == 0 and tt == 0:
                            D("sync", dbg["d_x2"], xb[0][:], reads=["xb0"], is_output=True)
                    if not debug:
                        if tt == 0 or PEER_ON:
                            D("sync", gfin_u, g_final.partition_broadcast(P), writes=["uT"])
                        rms_stats(0, "xb0")
                        E("vector", lambda e: e.scalar_tensor_tensor(
                            out=xb[0][:], in0=xb[0][:], scalar=rstd[0][:], in1=gfin_u, op0=ALU.mult, op1=ALU.mult),
                            ["xb0", "rstd0", "uT"], ["xb0"])
                        D("sync", y[t * P:(t + 1) * P, :], xb[0][:], reads=["xb0"], is_output=True)

        gfin_b = wa[0][:, 0:8, :].rearrange("p a b -> p (a b)")
        if debug:
            D("sync", gfin_b, g_final.partition_broadcast(P), writes=["wa0"])
        for t in range(NT if debug else 0):
            b = t % 2
            D("sync", xb[b][:], x[t * P:(t + 1) * P, :], writes=["xb%d" % b])
            rms_stats(b, "xb%d" % b)
            E("vector", lambda e, b=b: e.scalar_tensor_tensor(
                out=xb[b][:], in0=xb[b][:], scalar=rstd[b][:], in1=gfin_b, op0=ALU.mult, op1=ALU.mult),
                ["xb%d" % b, "rstd%d" % b, "wa0"], ["xb%d" % b])
            D("sync", y[t * P:(t + 1) * P, :], xb[b][:], reads=["xb%d" % b], is_output=True)
        pg.finish()
        nc._pg_stats = pg.stats
    return nc


def col_layout(v, ncol):
    return np.ascontiguousarray(np.asarray(v, np.float32).reshape(ncol, P).T)


def make_in_maps(inputs, cores):
    consts = host_consts()
    L = 0
    b_in = np.asarray(inputs["b_in"][L], np.float32)
    bcol = np.zeros((P, 56), np.float32)
    bcol[:, 0:52] = col_layout(b_in, 52)
    for g in range(4):
        hb = b_in[1024 + g * 64:1024 + (g + 1) * 64]
        bcol[:, 52 + g] = np.concatenate([hb, hb])
    f32c = lambda a: np.ascontiguousarray(a, dtype=np.float32)
    def gp_l(a):
        return f32c(np.asarray(a).reshape(32, 2, 64).transpose(1, 2, 0).reshape(P, 32))
    ssm_l = {
        "ssm_are_l": gp_l(inputs["ssm_A_re"][L]), "ssm_aim_l": gp_l(inputs["ssm_A_im"][L]),
        "ssm_ldt_l": gp_l(np.repeat(np.asarray(inputs["ssm_log_dt"][L])[:, None], 64, axis=1)),
        "ssm_bre_l": f32c(np.asarray(inputs["ssm_B_re"][L]).reshape(32, 2, 64, 16).transpose(1, 2, 0, 3).reshape(P, 32, 16)),
        "ssm_bim_l": f32c(np.asarray(inputs["ssm_B_im"][L]).reshape(32, 2, 64, 16).transpose(1, 2, 0, 3).reshape(P, 32, 16)),
        "ssm_cre_l": f32c(np.asarray(inputs["ssm_C_re"][L]).reshape(32, 2, 16, 64).transpose(1, 3, 0, 2).reshape(P, 32, 16)),
        "ssm_cim_l": f32c(np.asarray(inputs["ssm_C_im"][L]).reshape(32, 2, 16, 64).transpose(1, 3, 0, 2).reshape(P, 32, 16)),
        "ssm_d_col": col_layout(inputs["ssm_D"][L], 8), "b_glu_col": col_layout(inputs["b_glu"][L], 8),
        "w_glu": f32c(inputs["w_glu"][L]),
        "w_attn_branch": f32c(inputs["w_attn_branch"][L]), "w_ssm_branch": f32c(inputs["w_ssm_branch"][L]),
        "w_out": f32c(inputs["w_out"][L]),
        "g_ffn_col": col_layout(inputs["g_ffn"][L], KC),
        "w_query": f32c(inputs["w_query"][L]),
        "sub_keys_l": f32c(np.asarray(inputs["sub_keys"][L]).reshape(16, P, P).transpose(2, 0, 1)),
        "expert_down": f32c(inputs["expert_down"][L]), "expert_up": f32c(inputs["expert_up"][L]),
    }
    maps = []
    for c in cores:
        m = {
            "x": np.ascontiguousarray(inputs["x"][c], dtype=np.float32),
            "pos": np.ascontiguousarray(inputs["positions"][c], dtype=np.int32).reshape(1, SEQ),
            "c_col": col_layout(inputs["c"][c], KC),
            "w_ada": np.ascontiguousarray(inputs["w_ada"][L], dtype=np.float32),
            "b_ada_col": col_layout(inputs["b_ada"][L], 96),
            "g_mix_col": col_layout(inputs["g_mix"][L], KC),
            "w_in": np.ascontiguousarray(inputs["w_in"][L], dtype=np.float32),
            "b_in_col": bcol,
            "g_final": np.ascontiguousarray(inputs["g_final"], dtype=np.float32).reshape(1, D_MODEL),
            "attn_sinks": np.ascontiguousarray(inputs["attn_sinks"][L], dtype=np.float32).reshape(1, 16),
        }
        m.update(ssm_l)
        m.update(consts)
        maps.append(m)
    return maps


_NC_CACHE = {}


def kernel(**inputs):
    if "nc" not in _NC_CACHE:
        _NC_CACHE["nc"] = build_nc()
    nc = _NC_CACHE["nc"]
    cores = list(range(N_CORES))
    in_maps = make_in_maps(inputs, cores)
    res = run_bass_kernel_spmd(nc, in_maps, core_ids=cores)
    return np.stack([np.asarray(r["y"]) for r in res.results], axis=0).astype(np.float32)
```
